# Optimizing a Trainium2 kernel written in Bass

```python
import math
import jax, jax.numpy as jnp
from jax import lax
import numpy as np

D_MODEL = 1024
BATCH = 8
SEQ = 8192
DEPTH = 4

GRID_W = 64
CTX_LEN = 256
N_MIXERS = 3
EPS = 1e-6
HEAD_DIM = 128
N_HEADS = D_MODEL // HEAD_DIM
N_KV_HEADS = N_HEADS // 4
Q_GROUP = N_HEADS // N_KV_HEADS
ATTN_Q_W = N_HEADS * HEAD_DIM
ATTN_KV_W = N_KV_HEADS * HEAD_DIM
AXIS_DIM = HEAD_DIM // 2
ROPE_THETA = 10000.0
Q_BLOCK = 128
CHUNK = 128
GMLP_WIDTH = D_MODEL
GMLP_GROUPS = 8
GMLP_GROUP_DIM = GMLP_WIDTH // GMLP_GROUPS
HYENA_WIDTH = D_MODEL
HYENA_ORDER = 2
FILTER_EMB = 33
N_BANDS = (FILTER_EMB - 1) // 2
FILTER_HIDDEN = 64
DECAY_TARGET = 1e-2
FAST_DECAY_PCT = 0.3
SLOW_DECAY_PCT = 1.5
MAX_DECAY = math.log(DECAY_TARGET) / FAST_DECAY_PCT
MIN_DECAY = math.log(DECAY_TARGET) / SLOW_DECAY_PCT
N_EXPERTS = 32
TOP_K = 4
D_FF = D_MODEL
SWIGLU_ALPHA = 1.702
SWIGLU_LIMIT = 7.0
EXPERT_BLOCK = 256

kernel_name = 'hybrid_gqa_gmlp_hyena_moe_dit'


def rmsnorm(x, g):
    xf = x.astype(jnp.float32)
    y = xf * lax.rsqrt(jnp.mean(xf * xf, axis=-1, keepdims=True) + EPS)
    return y.astype(x.dtype) * g


def axial_rope(n_tok):
    rows = n_tok // GRID_W
    row = jnp.repeat(jnp.arange(rows, dtype=jnp.float32), GRID_W)
    col = jnp.tile(jnp.arange(GRID_W, dtype=jnp.float32), rows)
    inv = ROPE_THETA ** (-jnp.arange(0, AXIS_DIM, 2, dtype=jnp.float32) / AXIS_DIM)
    ang_r = row[:, None] * inv
    ang_c = col[:, None] * inv
    ang = jnp.concatenate([ang_r, ang_r, ang_c, ang_c], axis=-1)
    return jnp.cos(ang), jnp.sin(ang)


def apply_rope(x, cos, sin):
    xr = x.reshape(*x.shape[:-1], 2, 2, AXIS_DIM // 2)
    rot = jnp.stack([-xr[..., 1, :], xr[..., 0, :]], axis=-2).reshape(x.shape)
    return x * cos[None, :, None, :].astype(x.dtype) + rot * sin[None, :, None, :].astype(x.dtype)


def gqa_attend(q, k, v):
    s = jnp.einsum('bqhgd,bkhd->bhgqk', q, k).astype(jnp.float32) * (HEAD_DIM ** -0.5)
    p = jax.nn.softmax(s, axis=-1).astype(v.dtype)
    return jnp.einsum('bhgqk,bkhd->bqhgd', p, v)


def attention_mixer(h_lat, h_ctx, w_in, q_gain, k_gain, cos, sin, ctx_queries):
    B, S, _ = h_lat.shape
    C = h_ctx.shape[1]

    def heads_kv(p_kv):
        b, l, _ = p_kv.shape
        k = rmsnorm(p_kv[..., :ATTN_KV_W].reshape(b, l, N_KV_HEADS, HEAD_DIM), k_gain)
        v = p_kv[..., ATTN_KV_W:].reshape(b, l, N_KV_HEADS, HEAD_DIM)
        return k, v

    def heads_q(p_q):
        b, l, _ = p_q.shape
        return rmsnorm(p_q.reshape(b, l, N_HEADS, HEAD_DIM), q_gain)

    p_lat = h_lat @ w_in
    q_lat = apply_rope(heads_q(p_lat[..., :ATTN_Q_W]), cos, sin)
    k_lat, v_lat = heads_kv(p_lat[..., ATTN_Q_W:])
    k_lat = apply_rope(k_lat, cos, sin)
    if ctx_queries:
        p_ctx = h_ctx @ w_in
        q_ctx = heads_q(p_ctx[..., :ATTN_Q_W])
        k_ctx, v_ctx = heads_kv(p_ctx[..., ATTN_Q_W:])
    else:
        k_ctx, v_ctx = heads_kv(h_ctx @ w_in[:, ATTN_Q_W:])
    k_all = jnp.concatenate([k_lat, k_ctx], axis=1)
    v_all = jnp.concatenate([v_lat, v_ctx], axis=1)
    n_blk = S // Q_BLOCK
    q_blocks = q_lat.reshape(B, n_blk, Q_BLOCK, N_KV_HEADS, Q_GROUP, HEAD_DIM).transpose(1, 0, 2, 3, 4, 5)
    o = lax.map(lambda qb: gqa_attend(qb, k_all, v_all), q_blocks)
    y_lat = o.transpose(1, 0, 2, 3, 4, 5).reshape(B, S, ATTN_Q_W)
    y_ctx = None
    if ctx_queries:
        y_ctx = gqa_attend(q_ctx.reshape(B, C, N_KV_HEADS, Q_GROUP, HEAD_DIM), k_ctx, v_ctx).reshape(B, C, ATTN_Q_W)
    return y_lat, y_ctx


def gmlp_mixer(h, w_in, v_gain, w_s, b_s):
    B, L, _ = h.shape
    z = jax.nn.gelu(h @ w_in, approximate=False)
    u, v = jnp.split(z, 2, axis=-1)
    v = rmsnorm(v, v_gain).reshape(B, L // CHUNK, CHUNK, GMLP_GROUPS, GMLP_GROUP_DIM)
    mixed = jnp.einsum('gpq,bnqgc->bnpgc', w_s, v) + b_s.T[None, None, :, :, None]
    return u * mixed.reshape(B, L, GMLP_WIDTH)


def short_conv(p, w, b):
    pp = jnp.pad(p, ((0, 0), (1, 1), (0, 0)))
    return pp[:, :-2] * w[0] + pp[:, 1:-1] * w[1] + pp[:, 2:] * w[2] + b


def hyena_filter(L, w1, b1, w2, b2, w3, b3, w4, b4, freq):
    t = jnp.linspace(0.0, 1.0, L, dtype=jnp.float32)[:, None]
    w = 2.0 * math.pi * jnp.arange(L, dtype=jnp.float32)[:, None] / L
    f = jnp.linspace(1e-4, N_BANDS - 1, N_BANDS, dtype=jnp.float32)[None, :]
    z = jnp.concatenate([t, jnp.cos(f * w), -jnp.sin(f * w)], axis=-1)
    a = jnp.sin(freq * (z @ w1 + b1))
    a = jnp.sin(freq * (a @ w2 + b2))
    a = jnp.sin(freq * (a @ w3 + b3))
    k = (a @ w4 + b4).astype(jnp.float32)
    deltas = jnp.abs(jnp.linspace(MIN_DECAY, MAX_DECAY, HYENA_WIDTH, dtype=jnp.float32))
    window = jnp.exp(-t * deltas[None, :])
    k_fwd = k[:, :HYENA_WIDTH] * window
    k_bwd = k[:, HYENA_WIDTH:] * window
    zero = jnp.zeros((1, HYENA_WIDTH), jnp.float32)
    return jnp.concatenate([k_fwd, zero, k_bwd[:0:-1]], axis=0)


def hyena_mixer(h, w_in, conv_w, conv_b, f_w1, f_b1, f_w2, f_b2, f_w3, f_b3, f_w4, f_b4, freq, d_skip):
    B, L, _ = h.shape
    p = short_conv(h @ w_in, conv_w, conv_b)
    x0, x1, v = jnp.split(p, HYENA_ORDER + 1, axis=-1)
    v = v * x1
    filt = hyena_filter(L, f_w1, f_b1, f_w2, f_b2, f_w3, f_b3, f_w4, f_b4, freq)
    vf = jnp.fft.rfft(v.astype(jnp.float32), n=2 * L, axis=1)
    kf = jnp.fft.rfft(filt, n=2 * L, axis=0)
    y = jnp.fft.irfft(vf * kf[None], n=2 * L, axis=1)[:, :L].astype(v.dtype)
    y = y + v * d_skip
    return y * x0


def expert_ffn(xb, w1, b1, w2, b2):
    hh = xb @ w1 + b1
    glu, lin = jnp.split(hh, 2, axis=-1)
    glu = jnp.minimum(glu, SWIGLU_LIMIT)
    lin = jnp.clip(lin, -SWIGLU_LIMIT, SWIGLU_LIMIT)
    return (glu * jax.nn.sigmoid(SWIGLU_ALPHA * glu) * (lin + 1.0)) @ w2 + b2


def moe(h, router_w, router_b, w1, b1, w2, b2):
    n_tok, d = h.shape
    logits = (h @ router_w + router_b).astype(jnp.float32)
    top_val, top_idx = lax.top_k(logits, TOP_K)
    gates = jax.nn.softmax(top_val, axis=-1).astype(h.dtype)
    n_assign = n_tok * TOP_K
    flat_e = top_idx.reshape(-1)
    order = jnp.argsort(flat_e)
    sorted_e = flat_e[order]
    counts = jnp.bincount(flat_e, length=N_EXPERTS)
    padded = (counts + EXPERT_BLOCK - 1) // EXPERT_BLOCK * EXPERT_BLOCK
    pad_end = jnp.cumsum(padded)
    pad_start = pad_end - padded
    start = jnp.cumsum(counts) - counts
    dest = pad_start[sorted_e] + (jnp.arange(n_assign) - start[sorted_e])
    n_blocks = -(-n_assign // EXPERT_BLOCK) + N_EXPERTS
    rows = n_blocks * EXPERT_BLOCK
    x_disp = jnp.zeros((rows, d), h.dtype).at[dest].set(h[order // TOP_K])
    block_e = jnp.minimum(jnp.searchsorted(pad_end, jnp.arange(n_blocks) * EXPERT_BLOCK, side='right'), N_EXPERTS - 1)

    def run_block(args):
        xb, e = args
        return expert_ffn(xb, w1[e], b1[e], w2[e], b2[e])

    y_disp = lax.map(run_block, (x_disp.reshape(n_blocks, EXPERT_BLOCK, d), block_e)).reshape(rows, d)
    y_assign = jnp.zeros((n_assign, d), h.dtype).at[order].set(y_disp[dest])
    return jnp.einsum('nkd,nk->nd', y_assign.reshape(n_tok, TOP_K, d), gates)


def setup_inputs(seed: int = 0) -> dict:
    key = jax.random.key(seed)
    ks = iter(jax.random.split(key, 40))

    def nrm(shape, scale):
        return jax.random.normal(next(ks), shape, jnp.float32) * scale

    n_a = len(range(0, DEPTH, N_MIXERS))
    n_b = len(range(1, DEPTH, N_MIXERS))
    n_c = len(range(2, DEPTH, N_MIXERS))
    D = D_MODEL
    return {
        'x': nrm((BATCH, SEQ, D), 1.0),
        'c': nrm((BATCH, D), 1.0),
        'ctx': nrm((BATCH, CTX_LEN, D), 1.0),
        'c_ctx': nrm((D,), 1.0),
        'ada_w': nrm((DEPTH, D, 6 * D), 0.5 * D ** -0.5),
        'ada_b': nrm((DEPTH, 6 * D), 0.02),
        'norm1_g': 1.0 + nrm((DEPTH, D), 0.02),
        'norm2_g': 1.0 + nrm((DEPTH, D), 0.02),
        'mix_w_out': nrm((DEPTH, D, D), D ** -0.5),
        'router_w': nrm((DEPTH, D, N_EXPERTS), D ** -0.5),
        'router_b': nrm((DEPTH, N_EXPERTS), 0.01),
        'exp_w1': nrm((DEPTH, N_EXPERTS, D, 2 * D_FF), D ** -0.5),
        'exp_b1': nrm((DEPTH, N_EXPERTS, 2 * D_FF), 0.01),
        'exp_w2': nrm((DEPTH, N_EXPERTS, D_FF, D), D_FF ** -0.5),
        'exp_b2': nrm((DEPTH, N_EXPERTS, D), 0.01),
        'attn_w_in': nrm((n_a, D, ATTN_Q_W + 2 * ATTN_KV_W), D ** -0.5),
        'attn_q_gain': 1.0 + nrm((n_a, HEAD_DIM), 0.02),
        'attn_k_gain': 1.0 + nrm((n_a, HEAD_DIM), 0.02),
        'gmlp_w_in': nrm((n_b, D, 2 * GMLP_WIDTH), D ** -0.5),
        'gmlp_v_gain': 1.0 + nrm((n_b, GMLP_WIDTH), 0.02),
        'gmlp_w_s': nrm((n_b, GMLP_GROUPS, CHUNK, CHUNK), CHUNK ** -0.5),
        'gmlp_b_s': 1.0 + nrm((n_b, GMLP_GROUPS, CHUNK), 0.02),
        'hyena_w_in': nrm((n_c, D, (HYENA_ORDER + 1) * HYENA_WIDTH), D ** -0.5),
        'hyena_conv_w': nrm((n_c, 3, (HYENA_ORDER + 1) * HYENA_WIDTH), 0.5),
        'hyena_conv_b': nrm((n_c, (HYENA_ORDER + 1) * HYENA_WIDTH), 0.01),
        'hyena_f_w1': nrm((n_c, FILTER_EMB, FILTER_HIDDEN), FILTER_EMB ** -0.5),
        'hyena_f_b1': nrm((n_c, FILTER_HIDDEN), 0.02),
        'hyena_f_w2': nrm((n_c, FILTER_HIDDEN, FILTER_HIDDEN), FILTER_HIDDEN ** -0.5),
        'hyena_f_b2': nrm((n_c, FILTER_HIDDEN), 0.02),
        'hyena_f_w3': nrm((n_c, FILTER_HIDDEN, FILTER_HIDDEN), FILTER_HIDDEN ** -0.5),
        'hyena_f_b3': nrm((n_c, FILTER_HIDDEN), 0.02),
        'hyena_f_w4': nrm((n_c, FILTER_HIDDEN, 2 * HYENA_WIDTH), 0.02 * FILTER_HIDDEN ** -0.5),
        'hyena_f_b4': nrm((n_c, 2 * HYENA_WIDTH), 0.002),
        'hyena_freq': 1.0 + nrm((n_c, FILTER_HIDDEN), 0.02),
        'hyena_d': nrm((n_c, HYENA_WIDTH), 0.5),
        'final_g': 1.0 + nrm((D,), 0.02),
    }


def reference(x, c, ctx, c_ctx, ada_w, ada_b, norm1_g, norm2_g, mix_w_out, router_w, router_b,
              exp_w1, exp_b1, exp_w2, exp_b2, attn_w_in, attn_q_gain, attn_k_gain,
              gmlp_w_in, gmlp_v_gain, gmlp_w_s, gmlp_b_s,
              hyena_w_in, hyena_conv_w, hyena_conv_b, hyena_f_w1, hyena_f_b1, hyena_f_w2, hyena_f_b2,
              hyena_f_w3, hyena_f_b3, hyena_f_w4, hyena_f_b4, hyena_freq, hyena_d, final_g):
    B, S, D = x.shape
    C = ctx.shape[1]
    cos, sin = axial_rope(S)
    x_lat, x_ctx = x, ctx
    for i in range(DEPTH):
        kind = i % N_MIXERS
        j = i // N_MIXERS
        last = i == DEPTH - 1
        mod_lat = jax.nn.silu(c) @ ada_w[i] + ada_b[i]
        mod_ctx = jax.nn.silu(c_ctx) @ ada_w[i] + ada_b[i]
        sh1, sc1, g1, sh2, sc2, g2 = jnp.split(mod_lat[:, None, :], 6, axis=-1)
        csh1, csc1, cg1, csh2, csc2, cg2 = jnp.split(mod_ctx, 6, axis=-1)
        h_lat = rmsnorm(x_lat, norm1_g[i]) * (1.0 + sc1) + sh1
        h_ctx = rmsnorm(x_ctx, norm1_g[i]) * (1.0 + csc1) + csh1
        if kind == 0:
            y_lat, y_ctx = attention_mixer(h_lat, h_ctx, attn_w_in[j], attn_q_gain[j], attn_k_gain[j],
                                           cos, sin, not last)
        elif kind == 1:
            y_lat = gmlp_mixer(h_lat, gmlp_w_in[j], gmlp_v_gain[j], gmlp_w_s[j], gmlp_b_s[j])
            y_ctx = None if last else gmlp_mixer(h_ctx, gmlp_w_in[j], gmlp_v_gain[j], gmlp_w_s[j], gmlp_b_s[j])
        else:
            hy = (hyena_w_in[j], hyena_conv_w[j], hyena_conv_b[j], hyena_f_w1[j], hyena_f_b1[j],
                  hyena_f_w2[j], hyena_f_b2[j], hyena_f_w3[j], hyena_f_b3[j], hyena_f_w4[j], hyena_f_b4[j],
                  hyena_freq[j], hyena_d[j])
            y_lat = hyena_mixer(h_lat, *hy)
            y_ctx = None if last else hyena_mixer(h_ctx, *hy)
        x_lat = x_lat + g1 * (y_lat @ mix_w_out[i])
        h2_lat = rmsnorm(x_lat, norm2_g[i]) * (1.0 + sc2) + sh2
        if last:
            out = moe(h2_lat.reshape(B * S, D), router_w[i], router_b[i], exp_w1[i], exp_b1[i], exp_w2[i], exp_b2[i])
            x_lat = x_lat + g2 * out.reshape(B, S, D)
        else:
            x_ctx = x_ctx + cg1 * (y_ctx @ mix_w_out[i])
            h2_ctx = rmsnorm(x_ctx, norm2_g[i]) * (1.0 + csc2) + csh2
            tokens = jnp.concatenate([h2_lat.reshape(B * S, D), h2_ctx.reshape(B * C, D)], axis=0)
            out = moe(tokens, router_w[i], router_b[i], exp_w1[i], exp_b1[i], exp_w2[i], exp_b2[i])
            x_lat = x_lat + g2 * out[:B * S].reshape(B, S, D)
            x_ctx = x_ctx + cg2 * out[B * S:].reshape(B, C, D)
    return rmsnorm(x_lat, final_g)
```

```python
import math
import contextlib
import numpy as np
import concourse.bass as bass
import concourse.mybir as mybir
from concourse.bass_utils import run_bass_kernel_spmd

F32 = mybir.dt.float32
BF16 = mybir.dt.bfloat16
I32 = mybir.dt.int32
AF = mybir.ActivationFunctionType
ALU = mybir.AluOpType
AX = mybir.AxisListType

SEM_LIMIT = 30000


class Buf:
    __slots__ = ("name", "w", "r")

    def __init__(self, name):
        self.name = name
        self.w = None
        self.r = {}


class Prog:
    ENGS = ("pe", "act", "dve", "pool", "sync")

    def __init__(self, nc, stack):
        self.nc = nc
        self.stack = stack
        self.ops = {e: [] for e in self.ENGS}
        self.cnt = {}
        self.dma_pool = {}
        self.dma_rr = {}
        self.waited = {e: {} for e in self.ENGS}
        self.nsem = 0
        self.all_tokens = []
        self.last_tok = {}
        self.dma_toks = {}
        for e in ("pe", "act", "dve", "pool"):
            self.cnt[e] = [self._newsem(e), 0]
        for e, n in (("sync", 24), ("pool", 8), ("act", 8)):
            self.dma_pool[e] = [[self._newsem(e + "d"), 0] for _ in range(n)]
            self.dma_rr[e] = 0

    def _newsem(self, tag):
        self.nsem += 1
        return self.stack.enter_context(self.nc.semaphore(f"s_{tag}_{self.nsem}"))

    def _deps(self, reads, writes):
        deps = []
        for b in reads:
            if b.w is not None:
                deps.append(b.w)
        for b in writes:
            if b.w is not None:
                deps.append(b.w)
            deps.extend(b.r.values())
        return deps

    def _commit(self, tok, reads, writes):
        for b in reads:
            b.r[tok[0].name] = tok
        for b in writes:
            b.w = tok
            b.r = {}

    def _filter_waits(self, eng, deps, skip_self_pe=True):
        waits = []
        wd = self.waited[eng]
        best = {}
        for (sem, val, deng) in deps:
            if eng == "pe" and deng == "pe":
                continue
            key = sem.name
            if wd.get(key, 0) >= val:
                continue
            if best.get(key, (None, 0))[1] < val:
                best[key] = (sem, val)
        for key, (sem, val) in best.items():
            wd[key] = val
            waits.append((sem, val))
        return waits

    def op(self, eng, fn, reads=(), writes=()):
        deps = self._deps(reads, writes)
        waits = self._filter_waits(eng, deps)
        c = self.cnt[eng]
        if c[1] >= SEM_LIMIT:
            c[0] = self._newsem(eng)
            c[1] = 0
        c[1] += 1
        tok = (c[0], c[1], eng)
        self.ops[eng].append((waits, fn, (c[0], 1)))
        self._commit(tok, reads, writes)
        self.last_tok[eng] = tok
        return tok

    def dma(self, out, in_, reads=(), writes=(), eng="sync", **kw):
        deps = self._deps(reads, writes)
        pool = self.dma_pool[eng]
        slot = pool[self.dma_rr[eng] % len(pool)]
        self.dma_rr[eng] += 1
        if slot[1] * 16 >= SEM_LIMIT:
            slot[0] = self._newsem(eng + "d")
            slot[1] = 0
        if slot[1] > 0:
            deps.append((slot[0], slot[1] * 16, "dma"))
        waits = self._filter_waits(eng, deps)
        slot[1] += 1
        tok = (slot[0], slot[1] * 16, "dma")
        self.ops[eng].append((waits, lambda e: e.dma_start(out=out, in_=in_, **kw), (slot[0], 16)))
        self._commit(tok, reads, writes)
        self.dma_toks[slot[0].name] = tok
        return tok

    def barrier(self):
        toks = list(self.last_tok.values()) + list(self.dma_toks.values())
        for eng in self.ENGS:
            waits = self._filter_waits(eng, toks)
            if waits:
                self.ops[eng].append((waits, None, None))

    def final_wait(self, eng="sync"):
        toks = list(self.last_tok.values()) + list(self.dma_toks.values())
        waits = self._filter_waits(eng, toks)
        self.ops[eng].append((waits, None, None))

    def emit(self):
        nc = self.nc
        ops = self.ops
        self.ops = {e: [] for e in self.ENGS}

        def run(engobj, lst):
            for waits, fn, inc in lst:
                for sem, val in waits:
                    engobj.wait_ge(sem, val)
                if fn is not None:
                    ins = fn(engobj)
                    ins.then_inc(inc[0], inc[1])

        with nc.Block() as block:
            @block.tensor
            def _(e):
                run(e, ops["pe"])

            @block.scalar
            def _(e):
                run(e, ops["act"])

            @block.vector
            def _(e):
                run(e, ops["dve"])

            @block.gpsimd
            def _(e):
                run(e, ops["pool"])

            @block.sync
            def _(e):
                run(e, ops["sync"])

    def I(self, eng, meth, *args, reads=(), writes=(), **kw):
        return self.op(eng, lambda e: getattr(e, meth)(*args, **kw),
                       reads=[t.b for t in reads], writes=[t.b for t in writes])

    def D(self, out, in_, reads=(), writes=(), eng="sync", **kw):
        return self.dma(out, in_, reads=[t.b for t in reads], writes=[t.b for t in writes], eng=eng, **kw)


class Tl:
    __slots__ = ("t", "b")

    def __init__(self, t, name):
        self.t = t
        self.b = Buf(name)

    def __getitem__(self, k):
        return self.t[k]


class Ctx:
    n = 0

    def __init__(self, nc, P):
        self.nc = nc
        self.P = P
        self.st = contextlib.ExitStack()

    def __enter__(self):
        self.st.__enter__()
        return self

    def __exit__(self, *a):
        self.P.barrier()
        self.P.emit()
        return self.st.__exit__(*a)

    def sb(self, name, shape, dt, n=1):
        out = []
        for i in range(n):
            Ctx.n += 1
            nm = f"{name}_{Ctx.n}"
            out.append(Tl(self.st.enter_context(self.nc.sbuf_tensor(nm, list(shape), dt)), nm))
        return out if n > 1 else out[0]

    def ps(self, name, n, shape=(128, 512), dt=F32):
        out = []
        for i in range(n):
            Ctx.n += 1
            nm = f"{name}_{Ctx.n}"
            out.append(Tl(self.st.enter_context(self.nc.psum_tensor(nm, list(shape), dt)), nm))
        return out


class RR:
    def __init__(self, lst):
        self.l = lst
        self.i = 0

    def nxt(self):
        t = self.l[self.i % len(self.l)]
        self.i += 1
        return t


D = 1024
S = 8192
C = 256
NT = S + C
DEPTH = 4
NE = 32
KC = 8
EPS = 1e-6
CH = 512
CHUNKS = [(i * CH, CH, 0) for i in range(S // CH)] + [(S, C, 1)]


def _consts():
    c = {}
    c["identf"] = np.eye(128, dtype=np.float32)
    rows = S // 64
    row = np.repeat(np.arange(rows, dtype=np.float32), 64)
    col = np.tile(np.arange(64, dtype=np.float32), rows)
    inv = (10000.0 ** (-np.arange(0, 64, 2, dtype=np.float32) / 64)).astype(np.float32)
    ang_r = row[:, None] * inv
    ang_c = col[:, None] * inv
    ang = np.concatenate([ang_r, ang_r, ang_c, ang_c], axis=-1)
    c["cosT"] = np.ascontiguousarray(np.cos(ang).T.astype(np.float32))
    c["sinT"] = np.ascontiguousarray(np.sin(ang).T.astype(np.float32))
    Pm = np.zeros((128, 128), np.float32)
    for a in range(2):
        for j in range(32):
            Pm[a * 64 + j, a * 64 + 32 + j] = -1.0
            Pm[a * 64 + 32 + j, a * 64 + j] = 1.0
    c["permT"] = np.ascontiguousarray(Pm.T)
    c["zN_lat"], c["tN_lat"] = _hyena_consts_nat(S)
    p = np.arange(128, dtype=np.float64)[:, None]
    f = np.arange(128, dtype=np.float64)[None, :]
    c["dftC"] = np.cos(2 * np.pi * p * f / 128).astype(np.float32)
    c["dftS"] = np.sin(2 * np.pi * p * f / 128).astype(np.float32)
    c["twC"] = np.tile(np.cos(2 * np.pi * p * f / (2 * S)), (1, 4)).astype(np.float32)
    c["twS"] = np.tile(np.sin(2 * np.pi * p * f / (2 * S)), (1, 4)).astype(np.float32)
    c["zH_ctx"], c["tH_ctx"] = _hyena_consts(C)
    min_decay = math.log(1e-2) / 1.5
    max_decay = math.log(1e-2) / 0.3
    deltas = np.abs(np.linspace(min_decay, max_decay, D, dtype=np.float32))
    c["negdelta"] = np.ascontiguousarray((-deltas).reshape(KC, 128).T.astype(np.float32))
    return c


CONST_SHAPES = {"identf": [128, 128], "cosT": [128, S], "sinT": [128, S], "permT": [128, 128],
                "zH_ctx": [33, 2 * C], "tH_ctx": [1, 2 * C], "negdelta": [128, KC],
                "zN_lat": [33, 2 * S], "tN_lat": [1, 2 * S], "dftC": [128, 128], "dftS": [128, 128], "twC": [128, 512], "twS": [128, 512]}

WEIGHT_SHAPES = {
    "ada_w": [DEPTH * D, 6 * D], "ada_b": [DEPTH, 6 * D], "norm1_g": [DEPTH, D], "norm2_g": [DEPTH, D],
    "mix_w_out": [DEPTH * D, D], "router_w": [DEPTH * D, NE], "router_b": [DEPTH, NE],
    "exp_w1": [DEPTH * NE * D, 2 * D], "exp_b1": [DEPTH * NE, 2 * D], "exp_w2": [DEPTH * NE * D, D],
    "exp_b2": [DEPTH * NE, D], "attn_w_in": [2 * D, 1536], "attn_q_gain": [2, 128], "attn_k_gain": [2, 128],
    "gmlp_w_in": [D, 2 * D], "gmlp_v_gain": [1, D], "gmlp_w_s": [8 * 128, 128], "gmlp_b_s": [1, 1024],
    "hyena_w_in": [D, 3 * D], "hyena_conv_w": [3, 3 * D], "hyena_conv_b": [1, 3 * D],
    "hyena_f_w1": [33, 64], "hyena_f_b1": [1, 64], "hyena_f_w2": [64, 64], "hyena_f_b2": [1, 64],
    "hyena_f_w3": [64, 64], "hyena_f_b3": [1, 64], "hyena_f_w4": [64, 2 * D], "hyena_f_b4": [1, 2 * D],
    "hyena_freq": [1, 64], "hyena_d": [1, D], "final_g": [1, D],
}


class G:
    pass


def rows_to_cols(P, cx, g, src_ap, R, ncols, dst, psum):
    nch = ncols // 128
    rows = cx.sb("r2c", [R, ncols], F32)
    P.D(rows[:], src_ap, writes=[rows])
    done = 0
    while done < nch:
        n = min(nch - done, 512 // R)
        for j in range(n):
            P.I("pe", "transpose", psum[:, j * R:(j + 1) * R], rows[0:R, (done + j) * 128:(done + j + 1) * 128],
                g.ident[0:R, 0:R], reads=[rows, g.ident], writes=[psum])
        P.I("dve", "tensor_copy", dst[:, done:done + n, :], psum[:, 0:n * R].rearrange("p (n r) -> p n r", r=R),
            reads=[psum], writes=[dst])
        done += n


def norm_mod(P, g, xc, T, Aap, Bap, sq, rstd, tmp, hb, pss, fp32_to=None):
    P.I("act", "activation", sq[:, :, 0:T], xc[:, :, 0:T], AF.Square, reads=[xc], writes=[sq])
    for kc in range(KC):
        P.I("pe", "matmul", pss[:, 0:T], g.ones_b[:], sq[:, kc, 0:T], start=(kc == 0), stop=(kc == KC - 1),
            reads=[g.ones_b, sq], writes=[pss])
    P.I("act", "activation", rstd[:, 0:T], pss[:, 0:T], AF.Sqrt, bias=g.eps[:, 0:1], scale=1.0 / D,
        reads=[pss, g.eps], writes=[rstd])
    P.I("dve", "reciprocal", rstd[:, 0:T], rstd[:, 0:T], reads=[rstd], writes=[rstd])
    for kc in range(KC):
        P.I("dve", "scalar_tensor_tensor", tmp[:, kc, 0:T], xc[:, kc, 0:T], Aap(kc), rstd[:, 0:T], ALU.mult, ALU.mult,
            reads=[xc, rstd, g.modt], writes=[tmp])
    for kc in range(KC):
        if fp32_to is None:
            if Bap is None:
                P.I("act", "copy", hb[:, kc, 0:T], tmp[:, kc, 0:T], reads=[tmp], writes=[hb])
            else:
                P.I("act", "activation", hb[:, kc, 0:T], tmp[:, kc, 0:T], AF.Identity, bias=Bap(kc), scale=1.0,
                    reads=[tmp, g.modt], writes=[hb])
        else:
            P.I("act", "activation", tmp[:, kc, 0:T], tmp[:, kc, 0:T], AF.Identity, bias=Bap(kc), scale=1.0,
                reads=[tmp, g.modt], writes=[tmp])
    if fp32_to is not None:
        P.I("pool", "tensor_copy", hb[:, :, 0:T], tmp[:, :, 0:T], reads=[tmp], writes=[hb])


def xt_view(g, tok0, T):
    return g.xT[:, tok0:tok0 + T].rearrange("(k p) t -> p k t", p=128)


def stage_consts(P, cx, g):
    g.ident = cx.sb("ident", [128, 128], F32)
    P.D(g.ident[:], g.dr["identf"], writes=[g.ident])
    g.ones_b = cx.sb("ones_b", [128, 128], BF16)
    P.I("pool", "memset", g.ones_b[:], 1.0, writes=[g.ones_b])
    g.eps = cx.sb("eps", [128, 1], F32)
    P.I("pool", "memset", g.eps[:], EPS, writes=[g.eps])


def load_w_bf16(P, cx, src_rows_ap, ncols, name):
    w = cx.sb(name, [128, KC, ncols], BF16)
    P.D(w[:], src_rows_ap.rearrange("(k p) n -> p k n", p=128), writes=[w], eng="pool")
    return w


def out_proj_residual(P, g, l, r, wout, y, xc, T, psp):
    for mc in range(KC):
        ps = psp.nxt()
        for kc in range(KC):
            P.I("pe", "matmul", ps[:, 0:T], wout[:, kc, mc * 128:(mc + 1) * 128], y[:, kc, 0:T],
                start=(kc == 0), stop=(kc == KC - 1), reads=[wout, y], writes=[ps])
        P.I("dve", "scalar_tensor_tensor", xc[:, mc, 0:T], ps[:, 0:T], g.modt[:, l, 16 + mc, r:r + 1], xc[:, mc, 0:T],
            ALU.mult, ALU.add, reads=[ps, xc, g.modt], writes=[xc])


def stage_x_in(P, nc, g):
    with Ctx(nc, P) as cx:
        stage_consts(P, cx, g)
        xin = RR(cx.sb("xin", [128, 4, D], F32, n=2))
        xo = RR(cx.sb("xo", [128, KC, CH], F32, n=2))
        psp = RR(cx.ps("pst", 4))
        for ci, (tok0, T, r) in enumerate(CHUNKS):
            nt = T // 128
            xi = xin.nxt()
            src = g.dr["x"][tok0:tok0 + T, :] if r == 0 else g.dr["ctx"][:, :]
            P.D(xi[:, 0:nt, :], src.rearrange("(t p) d -> p t d", p=128), writes=[xi])
            o = xo.nxt()
            for kc in range(KC):
                ps = psp.nxt()
                for tt in range(nt):
                    P.I("pe", "transpose", ps[:, tt * 128:(tt + 1) * 128], xi[:, tt, kc * 128:(kc + 1) * 128], g.ident[:],
                        reads=[xi, g.ident], writes=[ps])
                P.I("act" if kc % 2 else "dve", "copy" if kc % 2 else "tensor_copy", o[:, kc, 0:T], ps[:, 0:T],
                    reads=[ps], writes=[o])
            P.D(xt_view(g, tok0, T), o[:, :, 0:T], reads=[o], writes=[g.xreg[ci]])


def stage_mods(P, nc, g, layers):
    with Ctx(nc, P) as cx:
        stage_consts(P, cx, g)
        psA = cx.ps("psA", 2)
        cv = cx.sb("cv", [2, D], F32)
        P.D(cv[:], g.dr["cvec"], writes=[cv])
        P.I("act", "activation", cv[:], cv[:], AF.Silu, reads=[cv], writes=[cv])
        cT = cx.sb("cT", [128, KC, 2], F32)
        for kc in range(KC):
            P.I("pe", "transpose", psA[0][:, kc * 2:kc * 2 + 2], cv[0:2, kc * 128:(kc + 1) * 128], g.ident[0:2, 0:2],
                reads=[cv, g.ident], writes=[psA[0]])
        P.I("dve", "tensor_copy", cT[:], psA[0][:, 0:16].rearrange("p (k r) -> p k r", r=2), reads=[psA[0]], writes=[cT])
        ng = cx.sb("ngT", [128, KC, 8], F32)
        rows = cx.sb("ngrows", [8, D], F32)
        P.D(rows[0:4, :], g.dr["norm1_g"], writes=[rows])
        P.D(rows[4:8, :], g.dr["norm2_g"], writes=[rows])
        for kc in range(KC):
            P.I("pe", "transpose", psA[1][:, kc * 8:kc * 8 + 8], rows[0:8, kc * 128:(kc + 1) * 128], g.ident[0:8, 0:8],
                reads=[rows, g.ident], writes=[psA[1]])
        P.I("dve", "tensor_copy", ng[:], psA[1][:, 0:64].rearrange("p (k r) -> p k r", r=8), reads=[psA[1]], writes=[ng])
        aw = RR(cx.sb("aw", [128, KC, 512], F32, n=3))
        ab = cx.sb("ab", [2, 6 * D], F32)
        md = cx.sb("md", [2, 6 * D], F32)
        psm = RR(cx.ps("psm", 2))
        pst = cx.ps("pstm", 1)[0]
        for l in layers:
            P.D(ab[:], g.dr["ada_b"][l:l + 1, :].partition_broadcast(2), writes=[ab])
            for cc in range(12):
                a = aw.nxt()
                P.D(a[:], g.dr["ada_w"][l * D:(l + 1) * D, cc * 512:(cc + 1) * 512].rearrange("(k p) n -> p k n", p=128),
                    writes=[a])
                ps = psm.nxt()
                for kc in range(KC):
                    P.I("pe", "matmul", ps[0:2, :], cT[:, kc, :], a[:, kc, :], start=(kc == 0), stop=(kc == KC - 1),
                        reads=[cT, a], writes=[ps])
                P.I("dve", "tensor_tensor", md[:, cc * 512:(cc + 1) * 512], ps[0:2, :], ab[:, cc * 512:(cc + 1) * 512], ALU.add,
                    reads=[ps, ab], writes=[md])
            for j in (1, 4):
                P.I("dve", "tensor_scalar", md[:, j * D:(j + 1) * D], md[:, j * D:(j + 1) * D], 1.0, None, ALU.add,
                    reads=[md], writes=[md])
            for q in range(48):
                P.I("pe", "transpose", pst[:, q * 2:q * 2 + 2], md[0:2, q * 128:(q + 1) * 128], g.ident[0:2, 0:2],
                    reads=[md, g.ident], writes=[pst])
            P.I("dve", "tensor_copy", g.modt[:, l, :, :], pst[:, 0:96].rearrange("p (q r) -> p q r", r=2),
                reads=[pst], writes=[g.modt])
            for r in range(2):
                P.I("dve", "tensor_tensor", g.modt[:, l, 8:16, r], g.modt[:, l, 8:16, r], ng[:, :, l], ALU.mult,
                    reads=[g.modt, ng], writes=[g.modt])
                P.I("dve", "tensor_tensor", g.modt[:, l, 32:40, r], g.modt[:, l, 32:40, r], ng[:, :, 4 + l], ALU.mult,
                    reads=[g.modt, ng], writes=[g.modt])
        fr = cx.sb("fgrow", [1, D], F32)
        P.D(fr[:], g.dr["final_g"], writes=[fr])
        for kc in range(KC):
            P.I("pe", "transpose", psA[0][:, 32 + kc:33 + kc], fr[0:1, kc * 128:(kc + 1) * 128], g.ident[0:1, 0:1],
                reads=[fr, g.ident], writes=[psA[0]])
        P.I("dve", "tensor_copy", g.fgT[:], psA[0][:, 32:40], reads=[psA[0]], writes=[g.fgT])


def stage_cast_experts(P, nc, g, layers):
    with Ctx(nc, P) as cx:
        fin = RR(cx.sb("cin", [128, 4, 2048], F32, n=3))
        fout = RR(cx.sb("cout", [128, 4, 2048], BF16, n=3))
        jn = [0]
        for l in layers:
            for e in range(NE):
                le = l * NE + e
                jobs = []
                for hk in range(2):
                    src = g.dr["exp_w1"][le * D + hk * 512: le * D + (hk + 1) * 512, :].rearrange("(k p) n -> p k n", p=128)
                    dst = g.w1b[l][e * D + hk * 512: e * D + (hk + 1) * 512, :].rearrange("(k p) n -> p k n", p=128)
                    jobs.append((src, dst, 2048))
                src = g.dr["exp_w2"][le * D:(le + 1) * D, :].rearrange("(k p) n -> p k n", p=128)
                dst = g.w2b[l][e * D:(e + 1) * D, :].rearrange("(k p) n -> p k n", p=128)
                jobs.append((src, dst, 1024))
                for (src, dst, nc_) in jobs:
                    a = fin.nxt()
                    b = fout.nxt()
                    if nc_ == 2048:
                        av, bv = a[:, :, :], b[:, :, :]
                    else:
                        av = a[:].rearrange("p k (h n) -> p (k h) n", h=2)
                        bv = b[:].rearrange("p k (h n) -> p (k h) n", h=2)
                    P.D(av, src, writes=[a])
                    eng, meth = (("act", "copy"), ("dve", "tensor_copy"), ("pool", "tensor_copy"))[jn[0] % 3]
                    jn[0] += 1
                    P.I(eng, meth, b[:], a[:], reads=[a], writes=[b])
                    P.D(dst, bv, reads=[b], writes=[g.wreg[l]], eng="act")


def stage_moe(P, nc, g, l, last):
    with Ctx(nc, P) as cx:
        stage_consts(P, cx, g)
        onesf = cx.sb("onesf", [32, 128], F32)
        P.I("pool", "memset", onesf[:], 1.0, writes=[onesf])
        selT = RR(cx.sb("selE", [32, 128], F32, n=2))
        rw = cx.sb("rw", [128, KC, NE], F32)
        P.D(rw[:], g.dr["router_w"][l * D:(l + 1) * D, :].rearrange("(k p) n -> p k n", p=128), writes=[rw])
        rb = cx.sb("rb", [128, NE], F32)
        P.D(rb[:], g.dr["router_b"][l:l + 1, :].partition_broadcast(128), writes=[rb])
        b2 = cx.sb("b2", [32, D], F32)
        P.D(b2[:], g.dr["exp_b2"][l * NE:(l + 1) * NE, :], writes=[b2])
        psr = RR(cx.ps("psr", 2))
        psw = RR(cx.ps("psw", 4))
        psy = RR(cx.ps("psy", 2))
        b1T = cx.sb("b1T", [128, 16, NE], F32)
        with Ctx(nc, P) as cx2:
            rows_to_cols(P, cx2, g, g.dr["exp_b1"][l * NE:(l + 1) * NE, :], NE, 2 * D, b1T, psr.nxt())
            P.I("dve", "tensor_scalar", b1T[:, 8:16, :], b1T[:, 8:16, :], 1.0, None, ALU.add, reads=[b1T], writes=[b1T])
        xc = cx.sb("xc", [128, KC, CH], F32)
        tmp = cx.sb("tmp", [128, KC, CH], F32)
        acc = cx.sb("acc", [128, KC, CH], F32)
        h2b = cx.sb("h2b", [128, KC, CH], BF16)
        act = RR(cx.sb("act", [128, KC, CH], BF16, n=2))
        rstd = cx.sb("rstd", [128, CH], F32)
        wt = RR(cx.sb("wt", [128, KC, D], BF16, n=6))
        gT = RR(cx.sb("g", [128, CH], F32, n=3))
        sT = RR(cx.sb("s", [128, CH], F32, n=3))
        lT = RR(cx.sb("lv", [128, CH], F32, n=3))
        gbcT = RR(cx.sb("gbc", [128, CH], F32, n=2))
        lg = cx.sb("lg", [128, 4, NE], F32)
        m8 = cx.sb("m8", [128, 4, 8], F32)
        msk = cx.sb("msk", [128, NE], F32)
        ex = cx.sb("ex", [128, NE], F32)
        sm = cx.sb("sm", [128, 4], F32)
        Gt = cx.sb("Gt", [128, 4, NE], F32)
        GT = cx.sb("GT", [32, CH], F32)

        def wload(e):
            le = l * NE + e
            t1, t2, t3 = wt.nxt(), wt.nxt(), wt.nxt()
            P.D(t1[:], g.w1b[l][e * D:(e + 1) * D, 0:D].rearrange("(k p) n -> p k n", p=128), reads=[g.wreg[l]], writes=[t1])
            P.D(t2[:], g.w1b[l][e * D:(e + 1) * D, D:2 * D].rearrange("(k p) n -> p k n", p=128), reads=[g.wreg[l]], writes=[t2])
            P.D(t3[:], g.w2b[l][e * D:(e + 1) * D, :].rearrange("(k p) n -> p k n", p=128), reads=[g.wreg[l]], writes=[t3])
            return t1, t2, t3

        chunks = [c for c in CHUNKS if not (last and c[2] == 1)]
        for (tok0, T, r) in chunks:
            ci = tok0 // CH
            nt = T // 128
            P.D(xc[:, :, 0:T], xt_view(g, tok0, T), reads=[g.xreg[ci]], writes=[xc])
            sq = act.l[0]
            norm_mod(P, g, xc, T, lambda kc: g.modt[:, l, 32 + kc, r:r + 1], lambda kc: g.modt[:, l, 24 + kc, r:r + 1],
                     sq, rstd, tmp, h2b, psr.nxt(), fp32_to=True)
            wnext = wload(0)
            psl = psr.nxt()
            for tt in range(nt):
                for kc in range(KC):
                    P.I("pe", "matmul", psl[:, tt * NE:(tt + 1) * NE], tmp[:, kc, tt * 128:(tt + 1) * 128], rw[:, kc, :],
                        start=(kc == 0), stop=(kc == KC - 1), reads=[tmp, rw], writes=[psl])
            psg = psr.nxt()
            for tt in range(nt):
                P.I("dve", "tensor_tensor", lg[:, tt, :], psl[:, tt * NE:(tt + 1) * NE], rb[:], ALU.add,
                    reads=[psl, rb], writes=[lg])
                P.I("dve", "max", m8[:, tt, :], lg[:, tt, :], reads=[lg], writes=[m8])
                P.I("dve", "tensor_scalar", msk[:], lg[:, tt, :], m8[:, tt, 3:4], None, ALU.is_ge, reads=[lg, m8], writes=[msk])
                P.I("dve", "tensor_scalar", sm[:, 0:1], m8[:, tt, 0:1], -1.0, None, ALU.mult, reads=[m8], writes=[sm])
                P.I("act", "activation", ex[:], lg[:, tt, :], AF.Exp, bias=sm[:, 0:1], scale=1.0, reads=[lg, sm], writes=[ex])
                P.I("dve", "tensor_tensor", ex[:], ex[:], msk[:], ALU.mult, reads=[ex, msk], writes=[ex])
                P.I("dve", "reduce_sum", sm[:, 1:2], ex[:], AX.X, reads=[ex], writes=[sm])
                P.I("dve", "reciprocal", sm[:, 2:3], sm[:, 1:2], reads=[sm], writes=[sm])
                P.I("dve", "tensor_scalar", Gt[:, tt, :], ex[:], sm[:, 2:3], None, ALU.mult, reads=[ex, sm], writes=[Gt])
                P.I("pe", "transpose", psg[0:NE, tt * 128:(tt + 1) * 128], Gt[:, tt, :], g.ident[:], reads=[Gt, g.ident], writes=[psg])
            P.I("act", "copy", GT[:, 0:T], psg[0:NE, 0:T], reads=[psg], writes=[GT])
            for mc in range(KC):
                ps = psy.nxt()
                P.I("pe", "matmul", ps[:, 0:T], b2[:, mc * 128:(mc + 1) * 128], GT[:, 0:T], start=True, stop=True,
                    reads=[b2, GT], writes=[ps])
                P.I("act", "copy", acc[:, mc, 0:T], ps[:, 0:T], reads=[ps], writes=[acc])
            def do_w2(a, w2):
                for mc in range(KC):
                    ps = psy.nxt()
                    for kc in range(KC):
                        P.I("pe", "matmul", ps[:, 0:T], w2[:, kc, mc * 128:(mc + 1) * 128], a[:, kc, 0:T],
                            start=(kc == 0), stop=(kc == KC - 1), reads=[w2, a], writes=[ps])
                    P.I("dve", "tensor_tensor", acc[:, mc, 0:T], acc[:, mc, 0:T], ps[:, 0:T], ALU.add, reads=[acc, ps], writes=[acc])

            prev = None
            for e in range(NE):
                w1g, w1l, w2 = wnext
                psb = psr.nxt()
                se = selT.nxt()
                P.I("dve", "tensor_scalar", se[:], onesf[:], g.ident[0:32, e:e + 1], None, ALU.mult, reads=[onesf, g.ident], writes=[se])
                P.I("pe", "matmul", psb[:, 0:T], se[:], GT[:, 0:T], start=True, stop=True,
                    reads=[se, GT], writes=[psb])
                gbc = gbcT.nxt()
                P.I("act", "copy", gbc[:, 0:T], psb[:, 0:T], reads=[psb], writes=[gbc])
                a = act.nxt()
                for f in range(KC):
                    pg, pl = psw.nxt(), psw.nxt()
                    for kc in range(KC):
                        P.I("pe", "matmul", pg[:, 0:T], w1g[:, kc, f * 128:(f + 1) * 128], h2b[:, kc, 0:T],
                            start=(kc == 0), stop=(kc == KC - 1), reads=[w1g, h2b], writes=[pg])
                    for kc in range(KC):
                        P.I("pe", "matmul", pl[:, 0:T], w1l[:, kc, f * 128:(f + 1) * 128], h2b[:, kc, 0:T],
                            start=(kc == 0), stop=(kc == KC - 1), reads=[w1l, h2b], writes=[pl])
                    gg, ss, ll = gT.nxt(), sT.nxt(), lT.nxt()
                    P.I("dve", "tensor_scalar", gg[:, 0:T], pg[:, 0:T], b1T[:, f, e:e + 1], 7.0, ALU.add, ALU.min,
                        reads=[pg, b1T], writes=[gg])
                    P.I("act", "activation", ss[:, 0:T], gg[:, 0:T], AF.Gelu_apprx_sigmoid, reads=[gg], writes=[ss])
                    P.I("dve", "tensor_scalar", ll[:, 0:T], pl[:, 0:T], b1T[:, 8 + f, e:e + 1], 8.0, ALU.add, ALU.min,
                        reads=[pl, b1T], writes=[ll])
                    P.I("dve", "scalar_tensor_tensor", ll[:, 0:T], ll[:, 0:T], -6.0, gbc[:, 0:T], ALU.max, ALU.mult,
                        reads=[ll, gbc], writes=[ll])
                    P.I("pool", "tensor_tensor", a[:, f, 0:T], ss[:, 0:T], ll[:, 0:T], ALU.mult, reads=[ss, ll], writes=[a])
                if prev is not None:
                    do_w2(*prev)
                prev = (a, w2)
                if e + 1 < NE:
                    wnext = wload(e + 1)
            do_w2(*prev)
            for kc in range(KC):
                P.I("dve", "scalar_tensor_tensor", xc[:, kc, 0:T], acc[:, kc, 0:T], g.modt[:, l, 40 + kc, r:r + 1], xc[:, kc, 0:T],
                    ALU.mult, ALU.add, reads=[acc, xc, g.modt], writes=[xc])
            P.D(xt_view(g, tok0, T), xc[:, :, 0:T], reads=[xc], writes=[g.xreg[ci]], eng="act")


def stage_final(P, nc, g):
    with Ctx(nc, P) as cx:
        stage_consts(P, cx, g)
        xc = RR(cx.sb("xc", [128, KC, CH], F32, n=2))
        tmp = cx.sb("tmp", [128, KC, CH], F32)
        sq = cx.sb("sq", [128, KC, CH], BF16)
        rstd = cx.sb("rstd", [128, CH], F32)
        ot = RR(cx.sb("ot", [128, 4, D], F32, n=2))
        pss = RR(cx.ps("pss", 2))
        pst = RR(cx.ps("pst", 4))
        for (tok0, T, r) in CHUNKS:
            if r == 1:
                continue
            ci = tok0 // CH
            x = xc.nxt()
            P.D(x[:, :, 0:T], xt_view(g, tok0, T), reads=[g.xreg[ci]], writes=[x])
            P.I("act", "activation", sq[:, :, 0:T], x[:, :, 0:T], AF.Square, reads=[x], writes=[sq])
            ps = pss.nxt()
            for kc in range(KC):
                P.I("pe", "matmul", ps[:, 0:T], g.ones_b[:], sq[:, kc, 0:T], start=(kc == 0), stop=(kc == KC - 1),
                    reads=[g.ones_b, sq], writes=[ps])
            P.I("act", "activation", rstd[:, 0:T], ps[:, 0:T], AF.Sqrt, bias=g.eps[:, 0:1], scale=1.0 / D, reads=[ps, g.eps], writes=[rstd])
            P.I("dve", "reciprocal", rstd[:, 0:T], rstd[:, 0:T], reads=[rstd], writes=[rstd])
            for kc in range(KC):
                P.I("dve", "scalar_tensor_tensor", tmp[:, kc, 0:T], x[:, kc, 0:T], g.fgT[:, kc:kc + 1], rstd[:, 0:T], ALU.mult, ALU.mult,
                    reads=[x, rstd, g.fgT], writes=[tmp])
            o = ot.nxt()
            for tt in range(T // 128):
                for half in range(2):
                    pt = pst.nxt()
                    for k4 in range(4):
                        kc = half * 4 + k4
                        P.I("pe", "transpose", pt[:, k4 * 128:(k4 + 1) * 128], tmp[:, kc, tt * 128:(tt + 1) * 128], g.ident[:],
                            reads=[tmp, g.ident], writes=[pt])
                    P.I("act" if half else "dve", "copy" if half else "tensor_copy", o[:, tt, half * 512:(half + 1) * 512], pt[:],
                        reads=[pt], writes=[o])
            P.D(g.out[tok0:tok0 + T, :].rearrange("(t p) d -> p t d", p=128), o[:, 0:T // 128, :], reads=[o], writes=[g.outreg])


def stage_attn(P, nc, g, l, j, last):
    SC = 128 ** -0.5
    with Ctx(nc, P) as cx:
        stage_consts(P, cx, g)
        wout = load_w_bf16(P, cx, g.dr["mix_w_out"][l * D:(l + 1) * D, :], D, "wout")
        KT = cx.sb("KT", [128, 2, NT], BF16)
        V = cx.sb("V", [128, NT // 128, 256], BF16)
        with Ctx(nc, P) as c1:
            permT = c1.sb("permT", [128, 128], F32)
            P.D(permT[:], g.dr["permT"], writes=[permT])
            win = load_w_bf16(P, c1, g.dr["attn_w_in"][j * D:(j + 1) * D, :], 1536, "win")
            gains = c1.sb("gains", [128, 2], F32)
            P.D(gains[:, 0:1], g.dr["attn_q_gain"][j:j + 1, :].rearrange("o d -> d o"), writes=[gains], allow_slow_non_contiguous=True)
            P.D(gains[:, 1:2], g.dr["attn_k_gain"][j:j + 1, :].rearrange("o d -> d o"), writes=[gains], allow_slow_non_contiguous=True)
            xc = c1.sb("xc", [128, KC, CH], F32)
            tmp = c1.sb("tmp", [128, KC, CH], F32)
            sq = c1.sb("sq", [128, KC, CH], BF16)
            hb = c1.sb("hb", [128, KC, CH], BF16)
            rstd = c1.sb("rstd", [128, CH], F32)
            cs = RR(c1.sb("cs", [128, 2, CH], F32, n=2))
            sqh = RR(c1.sb("sqh", [128, CH], BF16, n=2))
            rsh = RR(c1.sb("rsh", [128, CH], F32, n=2))
            kn = RR(c1.sb("kn", [128, CH], F32, n=2))
            t1 = RR(c1.sb("t1", [128, CH], F32, n=2))
            t2 = RR(c1.sb("t2", [128, CH], F32, n=2))
            qo = RR([c1.sb("qo", [128, KC, CH], BF16)])
            psa = RR(c1.ps("psa", 3))
            psb = RR(c1.ps("psb", 3))
            psn = RR(c1.ps("psn", 2))

            def head(src_col, T, gcol, rope, dst_ap, dst_tl, cst):
                ps = psa.nxt()
                for kc in range(KC):
                    P.I("pe", "matmul", ps[:, 0:T], win[:, kc, src_col:src_col + 128], hb[:, kc, 0:T],
                        start=(kc == 0), stop=(kc == KC - 1), reads=[win, hb], writes=[ps])
                s2 = sqh.nxt()
                P.I("act", "activation", s2[:, 0:T], ps[:, 0:T], AF.Square, reads=[ps], writes=[s2])
                p2 = psb.nxt()
                P.I("pe", "matmul", p2[:, 0:T], g.ones_b[:], s2[:, 0:T], start=True, stop=True, reads=[g.ones_b, s2], writes=[p2])
                rs = rsh.nxt()
                P.I("act", "activation", rs[:, 0:T], p2[:, 0:T], AF.Sqrt, bias=g.eps[:, 0:1], scale=1.0 / 128, reads=[p2, g.eps], writes=[rs])
                P.I("dve", "reciprocal", rs[:, 0:T], rs[:, 0:T], reads=[rs], writes=[rs])
                k = kn.nxt()
                P.I("dve", "scalar_tensor_tensor", k[:, 0:T], ps[:, 0:T], gains[:, gcol:gcol + 1], rs[:, 0:T], ALU.mult, ALU.mult,
                    reads=[ps, gains, rs], writes=[k])
                if not rope:
                    P.I("act", "copy", dst_ap, k[:, 0:T], reads=[k], writes=[dst_tl])
                    return
                p3 = psb.nxt()
                P.I("pe", "matmul", p3[:, 0:T], permT[:], k[:, 0:T], start=True, stop=True, reads=[permT, k], writes=[p3])
                a, b = t1.nxt(), t2.nxt()
                P.I("pool", "tensor_tensor", a[:, 0:T], k[:, 0:T], cst[:, 0, 0:T], ALU.mult, reads=[k, cst], writes=[a])
                P.I("dve", "tensor_tensor", b[:, 0:T], p3[:, 0:T], cst[:, 1, 0:T], ALU.mult, reads=[p3, cst], writes=[b])
                P.I("pool", "tensor_tensor", dst_ap, a[:, 0:T], b[:, 0:T], ALU.add, reads=[a, b], writes=[dst_tl])

            for (tok0, T, r) in CHUNKS:
                ci = tok0 // CH
                P.D(xc[:, :, 0:T], xt_view(g, tok0, T), reads=[g.xreg[ci]], writes=[xc])
                norm_mod(P, g, xc, T, lambda kc: g.modt[:, l, 8 + kc, r:r + 1], lambda kc: g.modt[:, l, 0 + kc, r:r + 1],
                         sq, rstd, tmp, hb, psn.nxt())
                cst = None
                if r == 0:
                    cst = cs.nxt()
                    P.D(cst[:, 0, 0:T], g.dr["cosT"][:, tok0:tok0 + T], writes=[cst])
                    P.D(cst[:, 1, 0:T], g.dr["sinT"][:, tok0:tok0 + T], writes=[cst])
                for hk in range(2):
                    head(1024 + hk * 128, T, 1, r == 0, KT[:, hk, tok0:tok0 + T], KT, cst)
                for tt in range(T // 128):
                    ps = psa.nxt()
                    for kc in range(KC):
                        P.I("pe", "matmul", ps[:, 0:256], hb[:, kc, tt * 128:(tt + 1) * 128], win[:, kc, 1280:1536],
                            start=(kc == 0), stop=(kc == KC - 1), reads=[hb, win], writes=[ps])
                    P.I("act", "copy", V[:, tok0 // 128 + tt, :], ps[:, 0:256], reads=[ps], writes=[V])
                if r == 1 and last:
                    continue
                q = qo.nxt()
                for h in range(8):
                    head(h * 128, T, 0, r == 0, q[:, h, 0:T], q, cst)
                P.D(g.qT[:, tok0:tok0 + T].rearrange("(k p) t -> p k t", p=128), q[:, :, 0:T], reads=[q], writes=[g.qreg[ci]], eng="act")

        with Ctx(nc, P) as c2:
            xc = RR(c2.sb("xc2", [128, KC, CH], F32, n=2))
            qc = RR(c2.sb("qc", [128, KC, CH], BF16, n=2))
            pT = RR(c2.sb("pT", [128, CH], BF16, n=4))
            y = RR(c2.sb("y", [128, KC, CH], BF16, n=2))
            rec = RR(c2.sb("rec", [128, CH], F32, n=2))
            pss = RR(c2.ps("pss", 3))
            pso = RR(c2.ps("pso", 2))
            psd = RR(c2.ps("psd", 2))
            pspj = RR(c2.ps("pspj", 1))
            for (tok0, T, r) in CHUNKS:
                if r == 1 and last:
                    continue
                ci = tok0 // CH
                kts = list(range(NT // 128)) if r == 0 else list(range(S // 128, NT // 128))
                x = xc.nxt()
                q = qc.nxt()
                P.D(q[:, :, 0:T], g.qT[:, tok0:tok0 + T].rearrange("(k p) t -> p k t", p=128), reads=[g.qreg[ci]], writes=[q])
                P.D(x[:, :, 0:T], xt_view(g, tok0, T), reads=[g.xreg[ci]], writes=[x])
                yy = y.nxt()
                for h in range(8):
                    kv = h // 4
                    po, pd = pso.nxt(), psd.nxt()
                    for ii, kt in enumerate(kts):
                        ps = pss.nxt()
                        P.I("pe", "matmul", ps[:, 0:T], KT[:, kv, kt * 128:(kt + 1) * 128], q[:, h, 0:T], start=True, stop=True,
                            reads=[KT, q], writes=[ps])
                        p = pT.nxt()
                        P.I("act", "activation", p[:, 0:T], ps[:, 0:T], AF.Exp, scale=SC, reads=[ps], writes=[p])
                        P.I("pe", "matmul", po[:, 0:T], V[:, kt, kv * 128:(kv + 1) * 128], p[:, 0:T], start=(ii == 0), stop=(ii == len(kts) - 1),
                            reads=[V, p], writes=[po])
                        P.I("pe", "matmul", pd[:, 0:T], g.ones_b[:], p[:, 0:T], start=(ii == 0), stop=(ii == len(kts) - 1),
                            reads=[g.ones_b, p], writes=[pd])
                    rc = rec.nxt()
                    P.I("dve", "reciprocal", rc[:, 0:T], pd[:, 0:T], reads=[pd], writes=[rc])
                    P.I("dve", "tensor_tensor", yy[:, h, 0:T], po[:, 0:T], rc[:, 0:T], ALU.mult, reads=[po, rc], writes=[yy])
                out_proj_residual(P, g, l, r, wout, yy, x, T, pspj)
                P.D(xt_view(g, tok0, T), x[:, :, 0:T], reads=[x], writes=[g.xreg[ci]], eng="act")


def stage_gmlp(P, nc, g, l, last):
    with Ctx(nc, P) as cx:
        stage_consts(P, cx, g)
        wout = load_w_bf16(P, cx, g.dr["mix_w_out"][l * D:(l + 1) * D, :], D, "wout")
        win = load_w_bf16(P, cx, g.dr["gmlp_w_in"], 2 * D, "win")
        psA = RR(cx.ps("psA", 3))
        psV = RR(cx.ps("psV", 2))
        psM = RR(cx.ps("psM", 2))
        pspj = RR(cx.ps("pspj", 1))
        wsT = cx.sb("wsT", [128, 8, 128], BF16)
        vgT = cx.sb("vgT", [128, 8, 1], F32)
        bsb = cx.sb("bsb", [128, D], F32)
        P.D(bsb[:], g.dr["gmlp_b_s"][0:1, :].partition_broadcast(128), writes=[bsb])
        with Ctx(nc, P) as c0:
            wsf = c0.sb("wsf", [128, 8, 128], F32)
            P.D(wsf[:], g.dr["gmlp_w_s"].rearrange("(g p) q -> p g q", p=128), writes=[wsf])
            for gi in range(8):
                ps = psA.nxt()
                P.I("pe", "transpose", ps[:, 0:128], wsf[:, gi, :], g.ident[:], reads=[wsf, g.ident], writes=[ps])
                P.I("act", "copy", wsT[:, gi, :], ps[:, 0:128], reads=[ps], writes=[wsT])
            rows_to_cols(P, c0, g, g.dr["gmlp_v_gain"], 1, D, vgT, psA.nxt())
        xc = cx.sb("xc", [128, KC, CH], F32)
        tmp = cx.sb("tmp", [128, KC, CH], F32)
        sq = cx.sb("sq", [128, KC, CH], BF16)
        hb = cx.sb("hb", [128, KC, CH], BF16)
        rstd = cx.sb("rstd", [128, CH], F32)
        uT = cx.sb("uT", [128, KC, CH], F32)
        vg = RR(cx.sb("vg", [128, D], F32, n=2))
        vn = RR(cx.sb("vn", [128, D], BF16, n=2))
        junk = cx.sb("junk", [128, D], BF16)
        st = RR(cx.sb("st", [128, 4], F32, n=2))
        mt = RR(cx.sb("mt", [128, 128], F32, n=2))
        y = cx.sb("y", [128, KC, CH], BF16)
        chunks = [c for c in CHUNKS if not (last and c[2] == 1)]
        for (tok0, T, r) in chunks:
            ci = tok0 // CH
            P.D(xc[:, :, 0:T], xt_view(g, tok0, T), reads=[g.xreg[ci]], writes=[xc])
            norm_mod(P, g, xc, T, lambda kc: g.modt[:, l, 8 + kc, r:r + 1], lambda kc: g.modt[:, l, 0 + kc, r:r + 1],
                     sq, rstd, tmp, hb, psA.nxt())
            for mc in range(KC):
                ps = psA.nxt()
                for kc in range(KC):
                    P.I("pe", "matmul", ps[:, 0:T], win[:, kc, mc * 128:(mc + 1) * 128], hb[:, kc, 0:T],
                        start=(kc == 0), stop=(kc == KC - 1), reads=[win, hb], writes=[ps])
                P.I("act", "activation", uT[:, mc, 0:T], ps[:, 0:T], AF.Gelu, reads=[ps], writes=[uT])
            for tt in range(T // 128):
                v = vg.nxt()
                for half in range(2):
                    ps = psV.nxt()
                    for kc in range(KC):
                        P.I("pe", "matmul", ps[:], hb[:, kc, tt * 128:(tt + 1) * 128], win[:, kc, D + half * 512:D + (half + 1) * 512],
                            start=(kc == 0), stop=(kc == KC - 1), reads=[hb, win], writes=[ps])
                    P.I("act", "activation", v[:, half * 512:(half + 1) * 512], ps[:], AF.Gelu, reads=[ps], writes=[v])
                s4 = st.nxt()
                P.I("dve", "memset", s4[:], 0.0, writes=[s4])
                P.I("act", "activation", junk[:], v[:], AF.Square, accum_out=s4[:, 0:1], reads=[v, s4], writes=[junk, s4])
                P.I("act", "activation", s4[:, 1:2], s4[:, 0:1], AF.Sqrt, bias=g.eps[:, 0:1], scale=1.0 / D, reads=[s4, g.eps], writes=[s4])
                P.I("dve", "reciprocal", s4[:, 2:3], s4[:, 1:2], reads=[s4], writes=[s4])
                n = vn.nxt()
                P.I("dve", "tensor_scalar", n[:], v[:], s4[:, 2:3], None, ALU.mult, reads=[v, s4], writes=[n])
                for half in range(2):
                    pm = psM.nxt()
                    for g4 in range(4):
                        gi = half * 4 + g4
                        P.I("pe", "matmul", pm[:, g4 * 128:(g4 + 1) * 128], n[:, gi * 128:(gi + 1) * 128], wsT[:, gi, :], start=True, stop=True,
                            reads=[n, wsT], writes=[pm])
                    for g4 in range(4):
                        gi = half * 4 + g4
                        m = mt.nxt()
                        P.I("dve", "scalar_tensor_tensor", m[:], pm[:, g4 * 128:(g4 + 1) * 128], vgT[:, gi, 0:1], bsb[:, gi * 128:(gi + 1) * 128],
                            ALU.mult, ALU.add, reads=[pm, vgT, bsb], writes=[m])
                        P.I("pool", "tensor_tensor", y[:, gi, tt * 128:(tt + 1) * 128], m[:], uT[:, gi, tt * 128:(tt + 1) * 128], ALU.mult,
                            reads=[m, uT], writes=[y])
            out_proj_residual(P, g, l, r, wout, y, xc, T, pspj)
            P.D(xt_view(g, tok0, T), xc[:, :, 0:T], reads=[xc], writes=[g.xreg[ci]], eng="act")


def build_program(layers=(0, 1, 2, 3), do_final=True, dbg_x=False, skip_moe=False, skip_mixer=False):
    nc = bass.Bass("TRN2", target_bir_lowering=False)
    g = G()
    g.dr = {}
    g.dr["x"] = nc.dram_tensor("x", [S, D], F32, kind="ExternalInput").ap()
    g.dr["ctx"] = nc.dram_tensor("ctx", [C, D], F32, kind="ExternalInput").ap()
    g.dr["cvec"] = nc.dram_tensor("cvec", [2, D], F32, kind="ExternalInput").ap()
    for k, shp in WEIGHT_SHAPES.items():
        g.dr[k] = nc.dram_tensor(k, list(shp), F32, kind="ExternalInput").ap()
    for k, shp in CONST_SHAPES.items():
        g.dr[k] = nc.dram_tensor(k, list(shp), F32, kind="ExternalInput").ap()
    g.out = nc.dram_tensor("out", [S, D], F32, kind="ExternalOutput").ap()
    if dbg_x:
        g.xT = nc.dram_tensor("xT_d", [D, NT], F32, kind="ExternalOutput").ap()
    else:
        g.xT = nc.dram_tensor("xT_d", [D, NT], F32).ap()
    g.qT = nc.dram_tensor("qT_d", [D, NT], BF16).ap()
    g.vT = nc.dram_tensor("vT_d", [D, NT], F32).ap()
    g.x0T = nc.dram_tensor("x0T_d", [D, NT], F32).ap()
    g.ycT = nc.dram_tensor("ycT_d", [D, NT], F32).ap()
    g.fT = nc.dram_tensor("fT_d", [D, 2 * S], F32).ap()
    g.freg = Tl(None, "freg")
    g.vreg = Tl(None, "vreg")
    g.yreg = Tl(None, "yreg")
    g.w1b = [nc.dram_tensor(f"w1b_d{l}", [NE * D, 2 * D], BF16).ap() if (l in layers and not skip_moe) else None for l in range(DEPTH)]
    g.w2b = [nc.dram_tensor(f"w2b_d{l}", [NE * D, D], BF16).ap() if (l in layers and not skip_moe) else None for l in range(DEPTH)]
    g.xreg = [Tl(None, f"xreg{i}") for i in range(len(CHUNKS))]
    g.qreg = [Tl(None, f"qreg{i}") for i in range(len(CHUNKS))]
    g.wreg = [Tl(None, f"wreg{i}") for i in range(DEPTH)]
    g.outreg = Tl(None, "outreg")
    with contextlib.ExitStack() as st:
        P = Prog(nc, st)
        g.modt = Tl(st.enter_context(nc.sbuf_tensor("modt", [128, DEPTH, 48, 2], F32)), "modt")
        g.fgT = Tl(st.enter_context(nc.sbuf_tensor("fgT", [128, KC], F32)), "fgT")
        stage_x_in(P, nc, g)
        stage_mods(P, nc, g, layers)
        if not skip_moe:
            stage_cast_experts(P, nc, g, layers)
        for l in layers:
            kind, j, last = l % 3, l // 3, l == DEPTH - 1
            if not skip_mixer:
                if kind == 0:
                    stage_attn(P, nc, g, l, j, last)
                elif kind == 1:
                    stage_gmlp(P, nc, g, l, last)
                else:
                    stage_hyena(P, nc, g, l, last)
            if not skip_moe:
                stage_moe(P, nc, g, l, last)
        if do_final:
            stage_final(P, nc, g)
        P.final_wait("sync")
        P.emit()
    return nc


def make_in_maps(inputs, cores):
    consts = _consts()
    shared = {}
    for k, shp in WEIGHT_SHAPES.items():
        shared[k] = np.ascontiguousarray(np.asarray(inputs[k], dtype=np.float32).reshape(shp))
    shared.update(consts)
    maps = []
    for b in cores:
        m = dict(shared)
        m["x"] = np.ascontiguousarray(inputs["x"][b])
        m["ctx"] = np.ascontiguousarray(inputs["ctx"][b])
        m["cvec"] = np.ascontiguousarray(np.stack([inputs["c"][b], inputs["c_ctx"]], axis=0).astype(np.float32))
        maps.append(m)
    return maps


def kernel(**inputs):
    nc = build_program()
    maps = make_in_maps(inputs, list(range(8)))
    res = run_bass_kernel_spmd(nc, maps, core_ids=list(range(8)))
    return np.stack([r["out"] for r in res.results], axis=0).astype(np.float32)


HCH = 510


def _hyena_consts(L):
    t = np.linspace(0.0, 1.0, L, dtype=np.float32)
    w = (2.0 * math.pi * np.arange(L, dtype=np.float32) / L).astype(np.float32)
    f = np.linspace(1e-4, 15, 16, dtype=np.float32)[None, :]
    z = np.concatenate([t[:, None], np.cos(f * w[:, None]), -np.sin(f * w[:, None])], axis=-1).astype(np.float32)
    pos = np.abs((L - 1) - np.arange(2 * L))
    pos = np.minimum(pos, L - 1)
    zH = np.ascontiguousarray(z[pos].T)
    tH = np.ascontiguousarray(t[pos][None, :])
    return zH, tH


def _hyena_consts_nat(L):
    t = np.linspace(0.0, 1.0, L, dtype=np.float32)
    w = (2.0 * math.pi * np.arange(L, dtype=np.float32) / L).astype(np.float32)
    f = np.linspace(1e-4, 15, 16, dtype=np.float32)[None, :]
    z = np.concatenate([t[:, None], np.cos(f * w[:, None]), -np.sin(f * w[:, None])], axis=-1).astype(np.float32)
    n = np.arange(2 * L)
    pos = np.where(n < L, n, 2 * L - n)
    pos = np.minimum(pos, L - 1)
    zN = np.ascontiguousarray(z[pos].T)
    tN = t[pos].copy()
    tN[L] = 1.0e4
    return zN, np.ascontiguousarray(tN[None, :])


def stage_hyena(P, nc, g, l, last):
    TWO_PI = 2.0 * math.pi
    with Ctx(nc, P) as cx:
        stage_consts(P, cx, g)
        with Ctx(nc, P) as c1:
            win = load_w_bf16(P, c1, g.dr["hyena_w_in"], 3 * D, "win")
            psA = RR(c1.ps("psA", 4))
            psn = RR(c1.ps("psn", 2))
            cwT = c1.sb("cwT", [128, 24, 3], F32)
            cbT = c1.sb("cbT", [128, 24, 1], F32)
            with Ctx(nc, P) as c0:
                rows_to_cols(P, c0, g, g.dr["hyena_conv_w"], 3, 3 * D, cwT, psA.nxt())
                rows_to_cols(P, c0, g, g.dr["hyena_conv_b"], 1, 3 * D, cbT, psA.nxt())
            xh = c1.sb("xh", [128, KC, 512], F32)
            tmp = c1.sb("tmp", [128, KC, 512], F32)
            sq = c1.sb("sq", [128, KC, 512], BF16)
            hb = c1.sb("hb", [128, KC, 512], BF16)
            rstd = c1.sb("rstd", [128, 512], F32)
            pc = RR(c1.sb("pc", [128, 512], F32, n=2))
            ca = RR(c1.sb("ca", [128, 512], F32, n=2))
            x1c = c1.sb("x1c", [128, KC, 512], F32)
            x0c = c1.sb("x0c", [128, KC, 512], F32)
            vvc = c1.sb("vvc", [128, KC, 512], F32)
            seqs = [(0, S, 0)] + ([] if last else [(S, C, 1)])
            for (s0, L, r) in seqs:
                tok0 = s0
                while tok0 < s0 + L:
                    T = min(HCH, s0 + L - tok0)
                    lo_ok = tok0 - 1 >= s0
                    hi_ok = tok0 + T < s0 + L
                    W = T + 2
                    c_lo = tok0 - 1 if lo_ok else tok0
                    c_hi = tok0 + T + 1 if hi_ok else tok0 + T
                    regs = [g.xreg[i] for i in range(len(CHUNKS)) if CHUNKS[i][0] < c_hi and CHUNKS[i][0] + CHUNKS[i][1] > c_lo]
                    if not (lo_ok and hi_ok):
                        P.I("pool", "memset", xh[:, :, 0:W], 1.0, writes=[xh])
                    P.D(xh[:, :, (c_lo - (tok0 - 1)):(c_hi - (tok0 - 1))], g.xT[:, c_lo:c_hi].rearrange("(k p) t -> p k t", p=128),
                        reads=regs, writes=[xh])
                    norm_mod(P, g, xh, W, lambda kc: g.modt[:, l, 8 + kc, r:r + 1], lambda kc: g.modt[:, l, 0 + kc, r:r + 1],
                             sq, rstd, tmp, hb, psn.nxt())
                    for m in list(range(8, 16)) + list(range(0, 8)) + list(range(16, 24)):
                        ps = psA.nxt()
                        for kc in range(KC):
                            P.I("pe", "matmul", ps[:, 0:W], win[:, kc, m * 128:(m + 1) * 128], hb[:, kc, 0:W],
                                start=(kc == 0), stop=(kc == KC - 1), reads=[win, hb], writes=[ps])
                        p = pc.nxt()
                        P.I("act", "copy", p[:, 0:W], ps[:, 0:W], reads=[ps], writes=[p])
                        if not lo_ok:
                            P.I("pool", "memset", p[:, 0:1], 0.0, writes=[p])
                        if not hi_ok:
                            P.I("pool", "memset", p[:, W - 1:W], 0.0, writes=[p])
                        a = ca.nxt()
                        P.I("dve", "tensor_scalar", a[:, 0:T], p[:, 0:T], cwT[:, m, 0:1], cbT[:, m, 0:1], ALU.mult, ALU.add,
                            reads=[p, cwT, cbT], writes=[a])
                        P.I("dve", "scalar_tensor_tensor", a[:, 0:T], p[:, 1:T + 1], cwT[:, m, 1:2], a[:, 0:T], ALU.mult, ALU.add,
                            reads=[p, cwT, a], writes=[a])
                        if m < 8:
                            dst, dtl = x0c[:, m, 0:T], x0c
                        elif m < 16:
                            dst, dtl = x1c[:, m - 8, 0:T], x1c
                        else:
                            dst, dtl = a[:, 0:T], a
                        P.I("dve", "scalar_tensor_tensor", dst, p[:, 2:T + 2], cwT[:, m, 2:3], a[:, 0:T], ALU.mult, ALU.add,
                            reads=[p, cwT, a], writes=[dtl])
                        if m >= 16:
                            P.I("pool", "tensor_tensor", vvc[:, m - 16, 0:T], a[:, 0:T], x1c[:, m - 16, 0:T], ALU.mult,
                                reads=[a, x1c], writes=[vvc])
                    P.D(g.vT[:, tok0:tok0 + T].rearrange("(k p) t -> p k t", p=128), vvc[:, :, 0:T], reads=[vvc], writes=[g.vreg], eng="act")
                    P.D(g.x0T[:, tok0:tok0 + T].rearrange("(k p) t -> p k t", p=128), x0c[:, :, 0:T], reads=[x0c], writes=[g.vreg], eng="act")
                    tok0 += T

        for (s0, L, r) in seqs:
            if r == 0:
                hyena_conv_fft(P, nc, g)
            else:
                hyena_conv_direct(P, nc, g, s0, L)

        with Ctx(nc, P) as c4:
            wout = load_w_bf16(P, c4, g.dr["mix_w_out"][l * D:(l + 1) * D, :], D, "wout")
            dT = c4.sb("dT", [128, KC, 1], F32)
            pspj = RR(c4.ps("pspj", 2))
            with Ctx(nc, P) as c0:
                rows_to_cols(P, c0, g, g.dr["hyena_d"], 1, D, dT, pspj.nxt())
            xc = RR(c4.sb("xc", [128, KC, CH], F32, n=2))
            yc = RR(c4.sb("ycc", [128, KC, CH], F32, n=2))
            vc = RR(c4.sb("vcc", [128, KC, CH], F32, n=2))
            x0 = RR(c4.sb("x0cc", [128, KC, CH], F32, n=2))
            y = RR(c4.sb("y", [128, KC, CH], BF16, n=2))
            for (tok0, T, r) in CHUNKS:
                if r == 1 and last:
                    continue
                ci = tok0 // CH
                x, yy, a_, b_, c_ = xc.nxt(), y.nxt(), yc.nxt(), vc.nxt(), x0.nxt()
                fm = lambda ap: ap[:, tok0:tok0 + T].rearrange("(k p) t -> p k t", p=128)
                P.D(x[:, :, 0:T], xt_view(g, tok0, T), reads=[g.xreg[ci]], writes=[x])
                P.D(a_[:, :, 0:T], fm(g.ycT), reads=[g.yreg], writes=[a_])
                P.D(b_[:, :, 0:T], fm(g.vT), reads=[g.vreg], writes=[b_])
                P.D(c_[:, :, 0:T], fm(g.x0T), reads=[g.vreg], writes=[c_])
                for kc in range(KC):
                    P.I("dve", "scalar_tensor_tensor", a_[:, kc, 0:T], b_[:, kc, 0:T], dT[:, kc, 0:1], a_[:, kc, 0:T], ALU.mult, ALU.add,
                        reads=[a_, b_, dT], writes=[a_])
                P.I("pool", "tensor_tensor", yy[:, :, 0:T], a_[:, :, 0:T], c_[:, :, 0:T], ALU.mult, reads=[a_, c_], writes=[yy])
                out_proj_residual(P, g, l, r, wout, yy, x, T, pspj)
                P.D(xt_view(g, tok0, T), x[:, :, 0:T], reads=[x], writes=[g.xreg[ci]], eng="act")


def hyena_filter_mlp(P, nc, g, c3, a3, zH_d, L2, CW, psF):
    TWO_PI = 2.0 * math.pi
    fw = [c3.sb("fw1", [33, 64], F32), c3.sb("fw2", [64, 64], F32), c3.sb("fw3", [64, 64], F32)]
    P.D(fw[0][:], g.dr["hyena_f_w1"], writes=[fw[0]])
    P.D(fw[1][:], g.dr["hyena_f_w2"], writes=[fw[1]])
    P.D(fw[2][:], g.dr["hyena_f_w3"], writes=[fw[2]])
    fb = c3.sb("fb", [64, 4], F32)
    for i, nm in enumerate(["hyena_f_b1", "hyena_f_b2", "hyena_f_b3", "hyena_freq"]):
        P.D(fb[:, i:i + 1], g.dr[nm].rearrange("o d -> d o"), writes=[fb], allow_slow_non_contiguous=True)
    P.I("dve", "tensor_scalar", fb[:, 3:4], fb[:, 3:4], 1.0 / TWO_PI, None, ALU.mult, reads=[fb], writes=[fb])
    zt = RR(c3.sb("zt", [33, CW], F32, n=2))
    u = RR(c3.sb("u", [64, CW], F32, n=2))
    ki = RR(c3.sb("ki", [64, CW], I32, n=2))
    kf = RR(c3.sb("kf", [64, CW], F32, n=2))
    cmpb = RR(c3.sb("cmpb", [64, CW], F32, n=2))
    av = RR(c3.sb("av", [64, CW], F32, n=2))
    for n0 in range(0, L2, CW):
        z = zt.nxt()
        P.D(z[:], zH_d[:, n0:n0 + CW], writes=[z])
        cur, K = z, 33
        for li in range(3):
            ps = psF.nxt()
            P.I("pe", "matmul", ps[0:64, 0:CW], fw[li][0:K, :], cur[0:K, :], start=True, stop=True, reads=[fw[li], cur], writes=[ps])
            uu, kk, ff, cc = u.nxt(), ki.nxt(), kf.nxt(), cmpb.nxt()
            P.I("dve", "tensor_scalar", uu[:], ps[0:64, 0:CW], fb[:, li:li + 1], fb[:, 3:4], ALU.add, ALU.mult, reads=[ps, fb], writes=[uu])
            P.I("dve", "tensor_copy", kk[:], uu[:], reads=[uu], writes=[kk])
            P.I("dve", "tensor_copy", ff[:], kk[:], reads=[kk], writes=[ff])
            P.I("dve", "tensor_tensor", uu[:], uu[:], ff[:], ALU.subtract, reads=[uu, ff], writes=[uu])
            P.I("dve", "tensor_scalar", cc[:], uu[:], 0.5, None, ALU.is_gt, reads=[uu], writes=[cc])
            P.I("dve", "tensor_tensor", uu[:], uu[:], cc[:], ALU.subtract, reads=[uu, cc], writes=[uu])
            P.I("dve", "tensor_scalar", cc[:], uu[:], -0.5, None, ALU.is_lt, reads=[uu], writes=[cc])
            P.I("dve", "tensor_tensor", uu[:], uu[:], cc[:], ALU.add, reads=[uu, cc], writes=[uu])
            if li < 2:
                nx = av.nxt()
                P.I("act", "activation", nx[:], uu[:], AF.Sin, scale=TWO_PI * (1.0 - 1e-6), reads=[uu], writes=[nx])
                cur, K = nx, 64
            else:
                P.I("act", "activation", a3[:, n0:n0 + CW], uu[:], AF.Sin, scale=TWO_PI * (1.0 - 1e-6), reads=[uu], writes=[a3])


def hyena_filter_chunk(P, g, psF, w4b, a3, b4T, ndl, tb, wn, tH_d, cc, n0, CW, L, dst_ap, dst_tl):
    half = 0 if n0 < L else 1
    ps = psF.nxt()
    P.I("pe", "matmul", ps[:, 0:CW], w4b[:, half * D + cc * 128: half * D + (cc + 1) * 128], a3[:, n0:n0 + CW],
        start=True, stop=True, reads=[w4b, a3], writes=[ps])
    t_ = tb.nxt()
    P.D(t_[:, 0:CW], tH_d[0:1, n0:n0 + CW].partition_broadcast(128), writes=[t_])
    w_ = wn.nxt()
    P.I("act", "activation", w_[:, 0:CW], t_[:, 0:CW], AF.Exp, scale=ndl[:, cc:cc + 1], reads=[t_, ndl], writes=[w_])
    P.I("dve", "scalar_tensor_tensor", dst_ap, ps[:, 0:CW], b4T[:, half * 8 + cc, 0:1], w_[:, 0:CW], ALU.add, ALU.mult,
        reads=[ps, b4T, w_], writes=[dst_tl])


def hyena_conv_direct(P, nc, g, s0, L):
    L2, CW = 2 * L, min(512, L)
    with Ctx(nc, P) as c2:
        psF = RR(c2.ps("psF", 4))
        a3 = c2.sb("a3", [64, L2], BF16)
        w4b = c2.sb("w4b", [64, 2 * D], BF16)
        P.D(w4b[:], g.dr["hyena_f_w4"], writes=[w4b], eng="pool")
        b4T = c2.sb("b4T", [128, 16, 1], F32)
        ndl = c2.sb("ndl", [128, KC], F32)
        P.D(ndl[:], g.dr["negdelta"], writes=[ndl])
        with Ctx(nc, P) as c0:
            rows_to_cols(P, c0, g, g.dr["hyena_f_b4"], 1, 2 * D, b4T, psF.nxt())
        with Ctx(nc, P) as c3:
            hyena_filter_mlp(P, nc, g, c3, a3, g.dr["zH_ctx"], L2, CW, psF)
        H = c2.sb("H", [128, L2], BF16)
        vt = c2.sb("vt", [128, L], BF16)
        junk = c2.sb("junk", [128, L], BF16)
        ycol = RR(c2.sb("ycol", [128, L], F32, n=2))
        tb = RR(c2.sb("tb", [128, CW], F32, n=2))
        wn = RR(c2.sb("wn", [128, CW], F32, n=2))
        for cc in range(KC):
            P.D(vt[:], g.vT[cc * 128:(cc + 1) * 128, s0:s0 + L], reads=[g.vreg], writes=[vt], eng="pool")
            for n0 in range(0, L2, CW):
                hyena_filter_chunk(P, g, psF, w4b, a3, b4T, ndl, tb, wn, g.dr["tH_ctx"], cc, n0, CW, L, H[:, n0:n0 + CW], H)
            yc_ = ycol.nxt()
            for t in range(L):
                P.I("dve", "scalar_tensor_tensor", junk[:], H[:, L - 1 - t:2 * L - 1 - t], 1.0, vt[:], ALU.mult, ALU.mult,
                    accum_out=yc_[:, t:t + 1], reads=[H, vt], writes=[junk, yc_])
            P.D(g.ycT[cc * 128:(cc + 1) * 128, s0:s0 + L], yc_[:], reads=[yc_], writes=[g.yreg], eng="act")


def hyena_conv_fft(P, nc, g):
    L, L2, CW = S, 2 * S, 512
    NFFT = float(L2)
    with Ctx(nc, P) as c2:
        psF = RR(c2.ps("psF", 4))
        a3 = c2.sb("a3", [64, L2], BF16)
        w4b = c2.sb("w4b", [64, 2 * D], BF16)
        P.D(w4b[:], g.dr["hyena_f_w4"], writes=[w4b], eng="pool")
        b4T = c2.sb("b4T", [128, 16, 1], F32)
        ndl = c2.sb("ndl", [128, KC], F32)
        P.D(ndl[:], g.dr["negdelta"], writes=[ndl])
        with Ctx(nc, P) as c0:
            rows_to_cols(P, c0, g, g.dr["hyena_f_b4"], 1, 2 * D, b4T, psF.nxt())
        with Ctx(nc, P) as c3:
            hyena_filter_mlp(P, nc, g, c3, a3, g.dr["zN_lat"], L2, CW, psF)
        Hc = RR(c2.sb("Hc", [128, 2048], F32, n=2))
        tb = RR(c2.sb("tb", [128, CW], F32, n=2))
        wn = RR(c2.sb("wn", [128, CW], F32, n=2))
        for cc in range(KC):
            for b0 in range(0, L2, 2048):
                h = Hc.nxt()
                for n0 in range(b0, b0 + 2048, CW):
                    hyena_filter_chunk(P, g, psF, w4b, a3, b4T, ndl, tb, wn, g.dr["tN_lat"], cc, n0, CW, L, h[:, n0 - b0:n0 - b0 + CW], h)
                P.D(g.fT[cc * 128:(cc + 1) * 128, b0:b0 + 2048], h[:], reads=[h], writes=[g.freg], eng="act")

    with Ctx(nc, P) as c5:
        cS = c5.sb("dftCf", [128, 2, 128], F32)
        P.D(cS[:, 0, :], g.dr["dftC"], writes=[cS])
        P.D(cS[:, 1, :], g.dr["dftS"], writes=[cS])
        F1 = c5.sb("F1", [128, 256], BF16)
        G1 = c5.sb("G1", [128, 256], BF16)
        G2 = c5.sb("G2", [128, 256], BF16)
        P.I("dve", "tensor_copy", F1[:, 0:128], cS[:, 0, :], reads=[cS], writes=[F1])
        P.I("dve", "tensor_scalar", F1[:, 128:256], cS[:, 1, :], -1.0, None, ALU.mult, reads=[cS], writes=[F1])
        P.I("dve", "tensor_copy", G1[:, 0:128], cS[:, 0, :], reads=[cS], writes=[G1])
        P.I("dve", "tensor_copy", G1[:, 128:256], cS[:, 1, :], reads=[cS], writes=[G1])
        P.I("dve", "tensor_scalar", G2[:, 0:128], cS[:, 1, :], -1.0, None, ALU.mult, reads=[cS], writes=[G2])
        P.I("dve", "tensor_copy", G2[:, 128:256], cS[:, 0, :], reads=[cS], writes=[G2])
        twC = c5.sb("twC", [128, 512], F32)
        twS = c5.sb("twS", [128, 512], F32)
        P.D(twC[:], g.dr["twC"], writes=[twC])
        P.D(twS[:], g.dr["twS"], writes=[twS])
        Cb, Sb, nSb, nSb2 = G1[:, 0:128], G1[:, 128:256], G2[:, 0:128], F1[:, 128:256]
        ps1 = RR(c5.ps("ps1", 3))
        ps3 = RR(c5.ps("ps3", 2))
        ps4 = RR(c5.ps("ps4", 2))
        ps6 = RR(c5.ps("ps6", 1))
        xf = c5.sb("xf", [64, 32, 128], F32)
        ff = c5.sb("ff", [128, 32, 128], F32)
        xb = RR(c5.sb("xb", [64, 32, 128], BF16, n=2))
        fbb = RR(c5.sb("fbb", [128, 32, 128], BF16, n=2))
        yo = RR(c5.sb("yo", [64, 32, 128], F32, n=2))
        m1 = RR(c5.sb("m1", [128, 512], F32, n=2))
        m2 = RR(c5.sb("m2", [128, 512], F32, n=2))
        pr = RR(c5.sb("pr", [128, 4, 128], BF16, n=3))
        pi = RR(c5.sb("pi", [128, 4, 128], BF16, n=3))
        kfr = RR(c5.sb("kfr", [128, 512], F32, n=2))
        kfi = RR(c5.sb("kfi", [128, 512], F32, n=2))
        ta = RR(c5.sb("ta", [128, 512], F32, n=2))
        tbb = RR(c5.sb("tbb", [128, 512], F32, n=2))
        tc = RR(c5.sb("tc", [128, 512], F32, n=2))
        td = RR(c5.sb("td", [128, 512], F32, n=2))

        def v4(ap):
            return ap.rearrange("p (c r f) -> p c r f", c=2, r=2)

        def twiddle(banks, outR, outI, inverse):
            for bi, ps in enumerate(banks):
                a, b = m1.nxt(), m2.nxt()
                P.I("dve", "tensor_tensor", a[:], ps[:, 0:512], twC[:], ALU.mult, reads=[ps, twC], writes=[a])
                P.I("dve", "tensor_tensor", b[:], ps[:, 0:512], twS[:], ALU.mult, reads=[ps, twS], writes=[b])
                av, bv = v4(a[:]), v4(b[:])
                oR, oI = outR[:, bi * 2:bi * 2 + 2, :], outI[:, bi * 2:bi * 2 + 2, :]
                if not inverse:
                    P.I("pool", "tensor_tensor", oR, av[:, :, 0, :], bv[:, :, 1, :], ALU.add, reads=[a, b], writes=[outR])
                    P.I("pool", "tensor_tensor", oI, av[:, :, 1, :], bv[:, :, 0, :], ALU.subtract, reads=[a, b], writes=[outI])
                else:
                    P.I("pool", "tensor_tensor", oR, av[:, :, 0, :], bv[:, :, 1, :], ALU.subtract, reads=[a, b], writes=[outR])
                    P.I("pool", "tensor_tensor", oI, bv[:, :, 0, :], av[:, :, 1, :], ALU.add, reads=[a, b], writes=[outI])

        def fwd(src, K, c0):
            banks = [ps1.nxt(), ps1.nxt()]
            for ch in range(4):
                P.I("pe", "matmul", banks[ch // 2][:, (ch % 2) * 256:(ch % 2) * 256 + 256], src[0:K, c0 + ch, :], F1[0:K, :],
                    start=True, stop=True, reads=[src, F1], writes=[banks[ch // 2]])
            R, I_ = pr.nxt(), pi.nxt()
            twiddle(banks, R, I_, False)
            Xr, Xi = ps3.nxt(), ps3.nxt()
            Rf, If = R[:].rearrange("p c f -> p (c f)"), I_[:].rearrange("p c f -> p (c f)")
            P.I("pe", "matmul", Xr[:, 0:512], Cb, Rf, start=True, stop=False, reads=[G1, R], writes=[Xr])
            P.I("pe", "matmul", Xr[:, 0:512], Sb, If, start=False, stop=True, reads=[G1, I_], writes=[Xr])
            P.I("pe", "matmul", Xi[:, 0:512], Cb, If, start=True, stop=False, reads=[G1, I_], writes=[Xi])
            P.I("pe", "matmul", Xi[:, 0:512], nSb, Rf, start=False, stop=True, reads=[G2, R], writes=[Xi])
            return Xr, Xi

        for sg in range(D // 32):
            c0 = sg * 32
            P.D(xf[:], g.vT[c0:c0 + 32, 0:S].rearrange("c (a b) -> a c b", b=128), reads=[g.vreg], writes=[xf])
            P.D(ff[:], g.fT[c0:c0 + 32, :].rearrange("c (a b) -> a c b", b=128), reads=[g.freg], writes=[ff])
            x_, f_, y_ = xb.nxt(), fbb.nxt(), yo.nxt()
            P.I("act", "copy", x_[:], xf[:], reads=[xf], writes=[x_])
            P.I("pool", "tensor_copy", f_[:], ff[:], reads=[ff], writes=[f_])
            for gq in range(8):
                Xr, Xi = fwd(f_, 128, gq * 4)
                kr, ki_ = kfr.nxt(), kfi.nxt()
                P.I("act", "activation", kr[:], Xr[:, 0:512], AF.Identity, scale=1.0 / NFFT, reads=[Xr], writes=[kr])
                P.I("act", "activation", ki_[:], Xi[:, 0:512], AF.Identity, scale=1.0 / NFFT, reads=[Xi], writes=[ki_])
                Xr, Xi = fwd(x_, 64, gq * 4)
                a, b, c_, d_ = ta.nxt(), tbb.nxt(), tc.nxt(), td.nxt()
                P.I("dve", "tensor_tensor", a[:], Xr[:, 0:512], kr[:], ALU.mult, reads=[Xr, kr], writes=[a])
                P.I("dve", "tensor_tensor", b[:], Xi[:, 0:512], ki_[:], ALU.mult, reads=[Xi, ki_], writes=[b])
                P.I("dve", "tensor_tensor", c_[:], Xr[:, 0:512], ki_[:], ALU.mult, reads=[Xr, ki_], writes=[c_])
                P.I("dve", "tensor_tensor", d_[:], Xi[:, 0:512], kr[:], ALU.mult, reads=[Xi, kr], writes=[d_])
                Yr, Yi = pr.nxt(), pi.nxt()
                P.I("pool", "tensor_tensor", Yr[:].rearrange("p c f -> p (c f)"), a[:], b[:], ALU.subtract, reads=[a, b], writes=[Yr])
                P.I("pool", "tensor_tensor", Yi[:].rearrange("p c f -> p (c f)"), c_[:], d_[:], ALU.add, reads=[c_, d_], writes=[Yi])
                banks = [ps4.nxt(), ps4.nxt()]
                for ch in range(4):
                    o = banks[ch // 2][:, (ch % 2) * 256:(ch % 2) * 256 + 256]
                    P.I("pe", "matmul", o, Yr[:, ch, :], G1[:], start=True, stop=False, reads=[Yr, G1], writes=[banks[ch // 2]])
                    P.I("pe", "matmul", o, Yi[:, ch, :], G2[:], start=False, stop=True, reads=[Yi, G2], writes=[banks[ch // 2]])
                Qr, Qi = pr.nxt(), pi.nxt()
                twiddle(banks, Qr, Qi, True)
                po = ps6.nxt()
                P.I("pe", "matmul", po[0:64, 0:512], G1[:, 0:64], Qr[:].rearrange("p c f -> p (c f)"), start=True, stop=False,
                    reads=[G1, Qr], writes=[po])
                P.I("pe", "matmul", po[0:64, 0:512], G2[:, 0:64], Qi[:].rearrange("p c f -> p (c f)"), start=False, stop=True,
                    reads=[G2, Qi], writes=[po])
                P.I("act", "copy", y_[:, gq * 4:gq * 4 + 4, :], po[0:64, 0:512].rearrange("p (c f) -> p c f", c=4), reads=[po], writes=[y_])
            P.D(g.ycT[c0:c0 + 32, 0:S].rearrange("c (a b) -> a c b", b=128), y_[:], reads=[y_], writes=[g.yreg], eng="act")
```

```python
import math
import contextlib
import numpy as np
import concourse.bass as bass
import concourse.mybir as mybir
from concourse.bass_utils import run_bass_kernel_spmd

F32 = mybir.dt.float32
BF16 = mybir.dt.bfloat16
I32 = mybir.dt.int32
AF = mybir.ActivationFunctionType
ALU = mybir.AluOpType
AX = mybir.AxisListType

SEM_LIMIT = 30000


class Buf:
    __slots__ = ("name", "w", "r")

    def __init__(self, name):
        self.name = name
        self.w = None
        self.r = {}


class Prog:
    ENGS = ("pe", "act", "dve", "pool", "sync")

    def __init__(self, nc, stack):
        self.nc = nc
        self.stack = stack
        self.ops = {e: [] for e in self.ENGS}
        self.cnt = {}
        self.dma_pool = {}
        self.dma_rr = {}
        self.waited = {e: {} for e in self.ENGS}
        self.nsem = 0
        self.all_tokens = []
        self.last_tok = {}
        self.dma_toks = {}
        for e in ("pe", "act", "dve", "pool"):
            self.cnt[e] = [self._newsem(e), 0]
        for e, n in (("sync", 24), ("pool", 8), ("act", 8)):
            self.dma_pool[e] = [[self._newsem(e + "d"), 0] for _ in range(n)]
            self.dma_rr[e] = 0

    def _newsem(self, tag):
        self.nsem += 1
        return self.stack.enter_context(self.nc.semaphore(f"s_{tag}_{self.nsem}"))

    def _deps(self, reads, writes):
        deps = []
        for b in reads:
            if b.w is not None:
                deps.append(b.w)
        for b in writes:
            if b.w is not None:
                deps.append(b.w)
            deps.extend(b.r.values())
        return deps

    def _commit(self, tok, reads, writes):
        for b in reads:
            b.r[tok[0].name] = tok
        for b in writes:
            b.w = tok
            b.r = {}

    def _filter_waits(self, eng, deps, skip_self_pe=True):
        waits = []
        wd = self.waited[eng]
        best = {}
        for (sem, val, deng) in deps:
            if eng == "pe" and deng == "pe":
                continue
            key = sem.name
            if wd.get(key, 0) >= val:
                continue
            if best.get(key, (None, 0))[1] < val:
                best[key] = (sem, val)
        for key, (sem, val) in best.items():
            wd[key] = val
            waits.append((sem, val))
        return waits

    def op(self, eng, fn, reads=(), writes=()):
        deps = self._deps(reads, writes)
        waits = self._filter_waits(eng, deps)
        c = self.cnt[eng]
        if c[1] >= SEM_LIMIT:
            c[0] = self._newsem(eng)
            c[1] = 0
        c[1] += 1
        tok = (c[0], c[1], eng)
        self.ops[eng].append((waits, fn, (c[0], 1)))
        self._commit(tok, reads, writes)
        self.last_tok[eng] = tok
        return tok

    def dma(self, out, in_, reads=(), writes=(), eng="sync", **kw):
        deps = self._deps(reads, writes)
        pool = self.dma_pool[eng]
        slot = pool[self.dma_rr[eng] % len(pool)]
        self.dma_rr[eng] += 1
        if slot[1] * 16 >= SEM_LIMIT:
            slot[0] = self._newsem(eng + "d")
            slot[1] = 0
        if slot[1] > 0:
            deps.append((slot[0], slot[1] * 16, "dma"))
        waits = self._filter_waits(eng, deps)
        slot[1] += 1
        tok = (slot[0], slot[1] * 16, "dma")
        self.ops[eng].append((waits, lambda e: e.dma_start(out=out, in_=in_, **kw), (slot[0], 16)))
        self._commit(tok, reads, writes)
        self.dma_toks[slot[0].name] = tok
        return tok

    def barrier(self):
        toks = list(self.last_tok.values()) + list(self.dma_toks.values())
        for eng in self.ENGS:
            waits = self._filter_waits(eng, toks)
            if waits:
                self.ops[eng].append((waits, None, None))

    def final_wait(self, eng="sync"):
        toks = list(self.last_tok.values()) + list(self.dma_toks.values())
        waits = self._filter_waits(eng, toks)
        self.ops[eng].append((waits, None, None))

    def emit(self):
        nc = self.nc
        ops = self.ops
        self.ops = {e: [] for e in self.ENGS}

        def run(engobj, lst):
            for waits, fn, inc in lst:
                for sem, val in waits:
                    engobj.wait_ge(sem, val)
                if fn is not None:
                    ins = fn(engobj)
                    ins.then_inc(inc[0], inc[1])

        with nc.Block() as block:
            @block.tensor
            def _(e):
                run(e, ops["pe"])

            @block.scalar
            def _(e):
                run(e, ops["act"])

            @block.vector
            def _(e):
                run(e, ops["dve"])

            @block.gpsimd
            def _(e):
                run(e, ops["pool"])

            @block.sync
            def _(e):
                run(e, ops["sync"])

    def I(self, eng, meth, *args, reads=(), writes=(), **kw):
        return self.op(eng, lambda e: getattr(e, meth)(*args, **kw),
                       reads=[t.b for t in reads], writes=[t.b for t in writes])

    def D(self, out, in_, reads=(), writes=(), eng="sync", **kw):
        return self.dma(out, in_, reads=[t.b for t in reads], writes=[t.b for t in writes], eng=eng, **kw)


class Tl:
    __slots__ = ("t", "b")

    def __init__(self, t, name):
        self.t = t
        self.b = Buf(name)

    def __getitem__(self, k):
        return self.t[k]


class Ctx:
    n = 0

    def __init__(self, nc, P):
        self.nc = nc
        self.P = P
        self.st = contextlib.ExitStack()

    def __enter__(self):
        self.st.__enter__()
        return self

    def __exit__(self, *a):
        self.P.barrier()
        self.P.emit()
        return self.st.__exit__(*a)

    def sb(self, name, shape, dt, n=1):
        out = []
        for i in range(n):
            Ctx.n += 1
            nm = f"{name}_{Ctx.n}"
            out.append(Tl(self.st.enter_context(self.nc.sbuf_tensor(nm, list(shape), dt)), nm))
        return out if n > 1 else out[0]

    def ps(self, name, n, shape=(128, 512), dt=F32):
        out = []
        for i in range(n):
            Ctx.n += 1
            nm = f"{name}_{Ctx.n}"
            out.append(Tl(self.st.enter_context(self.nc.psum_tensor(nm, list(shape), dt)), nm))
        return out


class RR:
    def __init__(self, lst):
        self.l = lst
        self.i = 0

    def nxt(self):
        t = self.l[self.i % len(self.l)]
        self.i += 1
        return t


D = 1024
S = 8192
C = 256
NT = S + C
DEPTH = 4
NE = 32
KC = 8
EPS = 1e-6
CH = 512
CHUNKS = [(i * CH, CH, 0) for i in range(S // CH)] + [(S, C, 1)]


def _consts():
    c = {}
    c["identf"] = np.eye(128, dtype=np.float32)
    rows = S // 64
    row = np.repeat(np.arange(rows, dtype=np.float32), 64)
    col = np.tile(np.arange(64, dtype=np.float32), rows)
    inv = (10000.0 ** (-np.arange(0, 64, 2, dtype=np.float32) / 64)).astype(np.float32)
    ang_r = row[:, None] * inv
    ang_c = col[:, None] * inv
    ang = np.concatenate([ang_r, ang_r, ang_c, ang_c], axis=-1)
    c["cosT"] = np.ascontiguousarray(np.cos(ang).T.astype(np.float32))
    c["sinT"] = np.ascontiguousarray(np.sin(ang).T.astype(np.float32))
    Pm = np.zeros((128, 128), np.float32)
    for a in range(2):
        for j in range(32):
            Pm[a * 64 + j, a * 64 + 32 + j] = -1.0
            Pm[a * 64 + 32 + j, a * 64 + j] = 1.0
    c["permT"] = np.ascontiguousarray(Pm.T)
    c["zN_lat"], c["tN_lat"] = _hyena_consts_nat(S)
    p = np.arange(128, dtype=np.float64)[:, None]
    f = np.arange(128, dtype=np.float64)[None, :]
    c["dftC"] = np.cos(2 * np.pi * p * f / 128).astype(np.float32)
    c["dftS"] = np.sin(2 * np.pi * p * f / 128).astype(np.float32)
    c["twC"] = np.tile(np.cos(2 * np.pi * p * f / (2 * S)), (1, 4)).astype(np.float32)
    c["twS"] = np.tile(np.sin(2 * np.pi * p * f / (2 * S)), (1, 4)).astype(np.float32)
    c["zH_ctx"], c["tH_ctx"] = _hyena_consts(C)
    min_decay = math.log(1e-2) / 1.5
    max_decay = math.log(1e-2) / 0.3
    deltas = np.abs(np.linspace(min_decay, max_decay, D, dtype=np.float32))
    c["negdelta"] = np.ascontiguousarray((-deltas).reshape(KC, 128).T.astype(np.float32))
    return c


CONST_SHAPES = {"identf": [128, 128], "cosT": [128, S], "sinT": [128, S], "permT": [128, 128],
                "zH_ctx": [33, 2 * C], "tH_ctx": [1, 2 * C], "negdelta": [128, KC],
                "zN_lat": [33, 2 * S], "tN_lat": [1, 2 * S], "dftC": [128, 128], "dftS": [128, 128], "twC": [128, 512], "twS": [128, 512]}

WEIGHT_SHAPES = {
    "ada_w": [DEPTH * D, 6 * D], "ada_b": [DEPTH, 6 * D], "norm1_g": [DEPTH, D], "norm2_g": [DEPTH, D],
    "mix_w_out": [DEPTH * D, D], "router_w": [DEPTH * D, NE], "router_b": [DEPTH, NE],
    "exp_w1": [DEPTH * NE * D, 2 * D], "exp_b1": [DEPTH * NE, 2 * D], "exp_w2": [DEPTH * NE * D, D],
    "exp_b2": [DEPTH * NE, D], "attn_w_in": [2 * D, 1536], "attn_q_gain": [2, 128], "attn_k_gain": [2, 128],
    "gmlp_w_in": [D, 2 * D], "gmlp_v_gain": [1, D], "gmlp_w_s": [8 * 128, 128], "gmlp_b_s": [1, 1024],
    "hyena_w_in": [D, 3 * D], "hyena_conv_w": [3, 3 * D], "hyena_conv_b": [1, 3 * D],
    "hyena_f_w1": [33, 64], "hyena_f_b1": [1, 64], "hyena_f_w2": [64, 64], "hyena_f_b2": [1, 64],
    "hyena_f_w3": [64, 64], "hyena_f_b3": [1, 64], "hyena_f_w4": [64, 2 * D], "hyena_f_b4": [1, 2 * D],
    "hyena_freq": [1, 64], "hyena_d": [1, D], "final_g": [1, D],
}


class G:
    pass


def rows_to_cols(P, cx, g, src_ap, R, ncols, dst, psum):
    nch = ncols // 128
    rows = cx.sb("r2c", [R, ncols], F32)
    P.D(rows[:], src_ap, writes=[rows])
    done = 0
    while done < nch:
        n = min(nch - done, 512 // R)
        for j in range(n):
            P.I("pe", "transpose", psum[:, j * R:(j + 1) * R], rows[0:R, (done + j) * 128:(done + j + 1) * 128],
                g.ident[0:R, 0:R], reads=[rows, g.ident], writes=[psum])
        P.I("dve", "tensor_copy", dst[:, done:done + n, :], psum[:, 0:n * R].rearrange("p (n r) -> p n r", r=R),
            reads=[psum], writes=[dst])
        done += n


def norm_mod(P, g, xc, T, Aap, Bap, sq, rstd, tmp, hb, pss, fp32_to=None):
    P.I("act", "activation", sq[:, :, 0:T], xc[:, :, 0:T], AF.Square, reads=[xc], writes=[sq])
    for kc in range(KC):
        P.I("pe", "matmul", pss[:, 0:T], g.ones_b[:], sq[:, kc, 0:T], start=(kc == 0), stop=(kc == KC - 1),
            reads=[g.ones_b, sq], writes=[pss])
    P.I("act", "activation", rstd[:, 0:T], pss[:, 0:T], AF.Sqrt, bias=g.eps[:, 0:1], scale=1.0 / D,
        reads=[pss, g.eps], writes=[rstd])
    P.I("dve", "reciprocal", rstd[:, 0:T], rstd[:, 0:T], reads=[rstd], writes=[rstd])
    for kc in range(KC):
        P.I("dve", "scalar_tensor_tensor", tmp[:, kc, 0:T], xc[:, kc, 0:T], Aap(kc), rstd[:, 0:T], ALU.mult, ALU.mult,
            reads=[xc, rstd, g.modt], writes=[tmp])
    for kc in range(KC):
        if fp32_to is None:
            if Bap is None:
                P.I("act", "copy", hb[:, kc, 0:T], tmp[:, kc, 0:T], reads=[tmp], writes=[hb])
            else:
                P.I("act", "activation", hb[:, kc, 0:T], tmp[:, kc, 0:T], AF.Identity, bias=Bap(kc), scale=1.0,
                    reads=[tmp, g.modt], writes=[hb])
        else:
            P.I("act", "activation", tmp[:, kc, 0:T], tmp[:, kc, 0:T], AF.Identity, bias=Bap(kc), scale=1.0,
                reads=[tmp, g.modt], writes=[tmp])
    if fp32_to is not None:
        P.I("pool", "tensor_copy", hb[:, :, 0:T], tmp[:, :, 0:T], reads=[tmp], writes=[hb])


def xt_view(g, tok0, T):
    return g.xT[:, tok0:tok0 + T].rearrange("(k p) t -> p k t", p=128)


def stage_consts(P, cx, g):
    g.ident = cx.sb("ident", [128, 128], F32)
    P.D(g.ident[:], g.dr["identf"], writes=[g.ident])
    g.ones_b = cx.sb("ones_b", [128, 128], BF16)
    P.I("pool", "memset", g.ones_b[:], 1.0, writes=[g.ones_b])
    g.eps = cx.sb("eps", [128, 1], F32)
    P.I("pool", "memset", g.eps[:], EPS, writes=[g.eps])


def load_w_bf16(P, cx, src_rows_ap, ncols, name):
    w = cx.sb(name, [128, KC, ncols], BF16)
    P.D(w[:], src_rows_ap.rearrange("(k p) n -> p k n", p=128), writes=[w], eng="pool")
    return w


def out_proj_residual(P, g, l, r, wout, y, xc, T, psp):
    for mc in range(KC):
        ps = psp.nxt()
        for kc in range(KC):
            P.I("pe", "matmul", ps[:, 0:T], wout[:, kc, mc * 128:(mc + 1) * 128], y[:, kc, 0:T],
                start=(kc == 0), stop=(kc == KC - 1), reads=[wout, y], writes=[ps])
        P.I("dve", "scalar_tensor_tensor", xc[:, mc, 0:T], ps[:, 0:T], g.modt[:, l, 16 + mc, r:r + 1], xc[:, mc, 0:T],
            ALU.mult, ALU.add, reads=[ps, xc, g.modt], writes=[xc])


def stage_x_in(P, nc, g):
    with Ctx(nc, P) as cx:
        stage_consts(P, cx, g)
        xin = RR(cx.sb("xin", [128, 4, D], F32, n=2))
        xo = RR(cx.sb("xo", [128, KC, CH], F32, n=2))
        psp = RR(cx.ps("pst", 4))
        for ci, (tok0, T, r) in enumerate(CHUNKS):
            nt = T // 128
            xi = xin.nxt()
            src = g.dr["x"][tok0:tok0 + T, :] if r == 0 else g.dr["ctx"][:, :]
            P.D(xi[:, 0:nt, :], src.rearrange("(t p) d -> p t d", p=128), writes=[xi])
            o = xo.nxt()
            for kc in range(KC):
                ps = psp.nxt()
                for tt in range(nt):
                    P.I("pe", "transpose", ps[:, tt * 128:(tt + 1) * 128], xi[:, tt, kc * 128:(kc + 1) * 128], g.ident[:],
                        reads=[xi, g.ident], writes=[ps])
                P.I("act" if kc % 2 else "dve", "copy" if kc % 2 else "tensor_copy", o[:, kc, 0:T], ps[:, 0:T],
                    reads=[ps], writes=[o])
            P.D(xt_view(g, tok0, T), o[:, :, 0:T], reads=[o], writes=[g.xreg[ci]])


def stage_mods(P, nc, g, layers):
    with Ctx(nc, P) as cx:
        stage_consts(P, cx, g)
        psA = cx.ps("psA", 2)
        cv = cx.sb("cv", [2, D], F32)
        P.D(cv[:], g.dr["cvec"], writes=[cv])
        P.I("act", "activation", cv[:], cv[:], AF.Silu, reads=[cv], writes=[cv])
        cT = cx.sb("cT", [128, KC, 2], F32)
        for kc in range(KC):
            P.I("pe", "transpose", psA[0][:, kc * 2:kc * 2 + 2], cv[0:2, kc * 128:(kc + 1) * 128], g.ident[0:2, 0:2],
                reads=[cv, g.ident], writes=[psA[0]])
        P.I("dve", "tensor_copy", cT[:], psA[0][:, 0:16].rearrange("p (k r) -> p k r", r=2), reads=[psA[0]], writes=[cT])
        ng = cx.sb("ngT", [128, KC, 8], F32)
        rows = cx.sb("ngrows", [8, D], F32)
        P.D(rows[0:4, :], g.dr["norm1_g"], writes=[rows])
        P.D(rows[4:8, :], g.dr["norm2_g"], writes=[rows])
        for kc in range(KC):
            P.I("pe", "transpose", psA[1][:, kc * 8:kc * 8 + 8], rows[0:8, kc * 128:(kc + 1) * 128], g.ident[0:8, 0:8],
                reads=[rows, g.ident], writes=[psA[1]])
        P.I("dve", "tensor_copy", ng[:], psA[1][:, 0:64].rearrange("p (k r) -> p k r", r=8), reads=[psA[1]], writes=[ng])
        aw = RR(cx.sb("aw", [128, KC, 512], F32, n=3))
        ab = cx.sb("ab", [2, 6 * D], F32)
        md = cx.sb("md", [2, 6 * D], F32)
        psm = RR(cx.ps("psm", 2))
        pst = cx.ps("pstm", 1)[0]
        for l in layers:
            P.D(ab[:], g.dr["ada_b"][l:l + 1, :].partition_broadcast(2), writes=[ab])
            for cc in range(12):
                a = aw.nxt()
                P.D(a[:], g.dr["ada_w"][l * D:(l + 1) * D, cc * 512:(cc + 1) * 512].rearrange("(k p) n -> p k n", p=128),
                    writes=[a])
                ps = psm.nxt()
                for kc in range(KC):
                    P.I("pe", "matmul", ps[0:2, :], cT[:, kc, :], a[:, kc, :], start=(kc == 0), stop=(kc == KC - 1),
                        reads=[cT, a], writes=[ps])
                P.I("dve", "tensor_tensor", md[:, cc * 512:(cc + 1) * 512], ps[0:2, :], ab[:, cc * 512:(cc + 1) * 512], ALU.add,
                    reads=[ps, ab], writes=[md])
            for j in (1, 4):
                P.I("dve", "tensor_scalar", md[:, j * D:(j + 1) * D], md[:, j * D:(j + 1) * D], 1.0, None, ALU.add,
                    reads=[md], writes=[md])
            for q in range(48):
                P.I("pe", "transpose", pst[:, q * 2:q * 2 + 2], md[0:2, q * 128:(q + 1) * 128], g.ident[0:2, 0:2],
                    reads=[md, g.ident], writes=[pst])
            P.I("dve", "tensor_copy", g.modt[:, l, :, :], pst[:, 0:96].rearrange("p (q r) -> p q r", r=2),
                reads=[pst], writes=[g.modt])
            for r in range(2):
                P.I("dve", "tensor_tensor", g.modt[:, l, 8:16, r], g.modt[:, l, 8:16, r], ng[:, :, l], ALU.mult,
                    reads=[g.modt, ng], writes=[g.modt])
                P.I("dve", "tensor_tensor", g.modt[:, l, 32:40, r], g.modt[:, l, 32:40, r], ng[:, :, 4 + l], ALU.mult,
                    reads=[g.modt, ng], writes=[g.modt])
        fr = cx.sb("fgrow", [1, D], F32)
        P.D(fr[:], g.dr["final_g"], writes=[fr])
        for kc in range(KC):
            P.I("pe", "transpose", psA[0][:, 32 + kc:33 + kc], fr[0:1, kc * 128:(kc + 1) * 128], g.ident[0:1, 0:1],
                reads=[fr, g.ident], writes=[psA[0]])
        P.I("dve", "tensor_copy", g.fgT[:], psA[0][:, 32:40], reads=[psA[0]], writes=[g.fgT])


def stage_cast_experts(P, nc, g, layers):
    with Ctx(nc, P) as cx:
        fin = RR(cx.sb("cin", [128, 4, 2048], F32, n=3))
        fout = RR(cx.sb("cout", [128, 4, 2048], BF16, n=3))
        jn = [0]
        for l in layers:
            for e in range(NE):
                le = l * NE + e
                jobs = []
                for hk in range(2):
                    src = g.dr["exp_w1"][le * D + hk * 512: le * D + (hk + 1) * 512, :].rearrange("(k p) n -> p k n", p=128)
                    dst = g.w1b[l][e * D + hk * 512: e * D + (hk + 1) * 512, :].rearrange("(k p) n -> p k n", p=128)
                    jobs.append((src, dst, 2048))
                src = g.dr["exp_w2"][le * D:(le + 1) * D, :].rearrange("(k p) n -> p k n", p=128)
                dst = g.w2b[l][e * D:(e + 1) * D, :].rearrange("(k p) n -> p k n", p=128)
                jobs.append((src, dst, 1024))
                for (src, dst, nc_) in jobs:
                    a = fin.nxt()
                    b = fout.nxt()
                    if nc_ == 2048:
                        av, bv = a[:, :, :], b[:, :, :]
                    else:
                        av = a[:].rearrange("p k (h n) -> p (k h) n", h=2)
                        bv = b[:].rearrange("p k (h n) -> p (k h) n", h=2)
                    P.D(av, src, writes=[a])
                    eng, meth = (("act", "copy"), ("dve", "tensor_copy"), ("pool", "tensor_copy"))[jn[0] % 3]
                    jn[0] += 1
                    P.I(eng, meth, b[:], a[:], reads=[a], writes=[b])
                    P.D(dst, bv, reads=[b], writes=[g.wreg[l]], eng="act")


def stage_moe(P, nc, g, l, last):
    with Ctx(nc, P) as cx:
        stage_consts(P, cx, g)
        onesf = cx.sb("onesf", [32, 128], F32)
        P.I("pool", "memset", onesf[:], 1.0, writes=[onesf])
        selT = RR(cx.sb("selE", [32, 128], F32, n=2))
        rw = cx.sb("rw", [128, KC, NE], F32)
        P.D(rw[:], g.dr["router_w"][l * D:(l + 1) * D, :].rearrange("(k p) n -> p k n", p=128), writes=[rw])
        rb = cx.sb("rb", [128, NE], F32)
        P.D(rb[:], g.dr["router_b"][l:l + 1, :].partition_broadcast(128), writes=[rb])
        b2 = cx.sb("b2", [32, D], F32)
        P.D(b2[:], g.dr["exp_b2"][l * NE:(l + 1) * NE, :], writes=[b2])
        psr = RR(cx.ps("psr", 2))
        psw = RR(cx.ps("psw", 4))
        psy = RR(cx.ps("psy", 2))
        b1T = cx.sb("b1T", [128, 16, NE], F32)
        with Ctx(nc, P) as cx2:
            rows_to_cols(P, cx2, g, g.dr["exp_b1"][l * NE:(l + 1) * NE, :], NE, 2 * D, b1T, psr.nxt())
            P.I("dve", "tensor_scalar", b1T[:, 8:16, :], b1T[:, 8:16, :], 1.0, None, ALU.add, reads=[b1T], writes=[b1T])
        xc = cx.sb("xc", [128, KC, CH], F32)
        tmp = cx.sb("tmp", [128, KC, CH], F32)
        acc = cx.sb("acc", [128, KC, CH], F32)
        h2b = cx.sb("h2b", [128, KC, CH], BF16)
        act = RR(cx.sb("act", [128, KC, CH], BF16, n=2))
        rstd = cx.sb("rstd", [128, CH], F32)
        wt = RR(cx.sb("wt", [128, KC, D], BF16, n=6))
        gT = RR(cx.sb("g", [128, CH], F32, n=3))
        sT = RR(cx.sb("s", [128, CH], F32, n=3))
        lT = RR(cx.sb("lv", [128, CH], F32, n=3))
        gbcT = RR(cx.sb("gbc", [128, CH], F32, n=2))
        lg = cx.sb("lg", [128, 4, NE], F32)
        m8 = cx.sb("m8", [128, 4, 8], F32)
        msk = cx.sb("msk", [128, NE], F32)
        ex = cx.sb("ex", [128, NE], F32)
        sm = cx.sb("sm", [128, 4], F32)
        Gt = cx.sb("Gt", [128, 4, NE], F32)
        GT = cx.sb("GT", [32, CH], F32)

        def wload(e):
            le = l * NE + e
            t1, t2, t3 = wt.nxt(), wt.nxt(), wt.nxt()
            P.D(t1[:], g.w1b[l][e * D:(e + 1) * D, 0:D].rearrange("(k p) n -> p k n", p=128), reads=[g.wreg[l]], writes=[t1])
            P.D(t2[:], g.w1b[l][e * D:(e + 1) * D, D:2 * D].rearrange("(k p) n -> p k n", p=128), reads=[g.wreg[l]], writes=[t2])
            P.D(t3[:], g.w2b[l][e * D:(e + 1) * D, :].rearrange("(k p) n -> p k n", p=128), reads=[g.wreg[l]], writes=[t3])
            return t1, t2, t3

        chunks = [c for c in CHUNKS if not (last and c[2] == 1)]
        for (tok0, T, r) in chunks:
            ci = tok0 // CH
            nt = T // 128
            P.D(xc[:, :, 0:T], xt_view(g, tok0, T), reads=[g.xreg[ci]], writes=[xc])
            sq = act.l[0]
            norm_mod(P, g, xc, T, lambda kc: g.modt[:, l, 32 + kc, r:r + 1], lambda kc: g.modt[:, l, 24 + kc, r:r + 1],
                     sq, rstd, tmp, h2b, psr.nxt(), fp32_to=True)
            wnext = wload(0)
            psl = psr.nxt()
            for tt in range(nt):
                for kc in range(KC):
                    P.I("pe", "matmul", psl[:, tt * NE:(tt + 1) * NE], tmp[:, kc, tt * 128:(tt + 1) * 128], rw[:, kc, :],
                        start=(kc == 0), stop=(kc == KC - 1), reads=[tmp, rw], writes=[psl])
            psg = psr.nxt()
            for tt in range(nt):
                P.I("dve", "tensor_tensor", lg[:, tt, :], psl[:, tt * NE:(tt + 1) * NE], rb[:], ALU.add,
                    reads=[psl, rb], writes=[lg])
                P.I("dve", "max", m8[:, tt, :], lg[:, tt, :], reads=[lg], writes=[m8])
                P.I("dve", "tensor_scalar", msk[:], lg[:, tt, :], m8[:, tt, 3:4], None, ALU.is_ge, reads=[lg, m8], writes=[msk])
                P.I("dve", "tensor_scalar", sm[:, 0:1], m8[:, tt, 0:1], -1.0, None, ALU.mult, reads=[m8], writes=[sm])
                P.I("act", "activation", ex[:], lg[:, tt, :], AF.Exp, bias=sm[:, 0:1], scale=1.0, reads=[lg, sm], writes=[ex])
                P.I("dve", "tensor_tensor", ex[:], ex[:], msk[:], ALU.mult, reads=[ex, msk], writes=[ex])
                P.I("dve", "reduce_sum", sm[:, 1:2], ex[:], AX.X, reads=[ex], writes=[sm])
                P.I("dve", "reciprocal", sm[:, 2:3], sm[:, 1:2], reads=[sm], writes=[sm])
                P.I("dve", "tensor_scalar", Gt[:, tt, :], ex[:], sm[:, 2:3], None, ALU.mult, reads=[ex, sm], writes=[Gt])
                P.I("pe", "transpose", psg[0:NE, tt * 128:(tt + 1) * 128], Gt[:, tt, :], g.ident[:], reads=[Gt, g.ident], writes=[psg])
            P.I("act", "copy", GT[:, 0:T], psg[0:NE, 0:T], reads=[psg], writes=[GT])
            for mc in range(KC):
                ps = psy.nxt()
                P.I("pe", "matmul", ps[:, 0:T], b2[:, mc * 128:(mc + 1) * 128], GT[:, 0:T], start=True, stop=True,
                    reads=[b2, GT], writes=[ps])
                P.I("act", "copy", acc[:, mc, 0:T], ps[:, 0:T], reads=[ps], writes=[acc])
            def do_w2(a, w2):
                for mc in range(KC):
                    ps = psy.nxt()
                    for kc in range(KC):
                        P.I("pe", "matmul", ps[:, 0:T], w2[:, kc, mc * 128:(mc + 1) * 128], a[:, kc, 0:T],
                            start=(kc == 0), stop=(kc == KC - 1), reads=[w2, a], writes=[ps])
                    P.I("dve", "tensor_tensor", acc[:, mc, 0:T], acc[:, mc, 0:T], ps[:, 0:T], ALU.add, reads=[acc, ps], writes=[acc])

            prev = None
            for e in range(NE):
                w1g, w1l, w2 = wnext
                psb = psr.nxt()
                se = selT.nxt()
                P.I("dve", "tensor_scalar", se[:], onesf[:], g.ident[0:32, e:e + 1], None, ALU.mult, reads=[onesf, g.ident], writes=[se])
                P.I("pe", "matmul", psb[:, 0:T], se[:], GT[:, 0:T], start=True, stop=True,
                    reads=[se, GT], writes=[psb])
                gbc = gbcT.nxt()
                P.I("act", "copy", gbc[:, 0:T], psb[:, 0:T], reads=[psb], writes=[gbc])
                a = act.nxt()
                for f in range(KC):
                    pg, pl = psw.nxt(), psw.nxt()
                    for kc in range(KC):
                        P.I("pe", "matmul", pg[:, 0:T], w1g[:, kc, f * 128:(f + 1) * 128], h2b[:, kc, 0:T],
                            start=(kc == 0), stop=(kc == KC - 1), reads=[w1g, h2b], writes=[pg])
                    for kc in range(KC):
                        P.I("pe", "matmul", pl[:, 0:T], w1l[:, kc, f * 128:(f + 1) * 128], h2b[:, kc, 0:T],
                            start=(kc == 0), stop=(kc == KC - 1), reads=[w1l, h2b], writes=[pl])
                    gg, ss, ll = gT.nxt(), sT.nxt(), lT.nxt()
                    P.I("dve", "tensor_scalar", gg[:, 0:T], pg[:, 0:T], b1T[:, f, e:e + 1], 7.0, ALU.add, ALU.min,
                        reads=[pg, b1T], writes=[gg])
                    P.I("act", "activation", ss[:, 0:T], gg[:, 0:T], AF.Gelu_apprx_sigmoid, reads=[gg], writes=[ss])
                    P.I("dve", "tensor_scalar", ll[:, 0:T], pl[:, 0:T], b1T[:, 8 + f, e:e + 1], 8.0, ALU.add, ALU.min,
                        reads=[pl, b1T], writes=[ll])
                    P.I("dve", "scalar_tensor_tensor", ll[:, 0:T], ll[:, 0:T], -6.0, gbc[:, 0:T], ALU.max, ALU.mult,
                        reads=[ll, gbc], writes=[ll])
                    P.I("pool", "tensor_tensor", a[:, f, 0:T], ss[:, 0:T], ll[:, 0:T], ALU.mult, reads=[ss, ll], writes=[a])
                if prev is not None:
                    do_w2(*prev)
                prev = (a, w2)
                if e + 1 < NE:
                    wnext = wload(e + 1)
            do_w2(*prev)
            for kc in range(KC):
                P.I("dve", "scalar_tensor_tensor", xc[:, kc, 0:T], acc[:, kc, 0:T], g.modt[:, l, 40 + kc, r:r + 1], xc[:, kc, 0:T],
                    ALU.mult, ALU.add, reads=[acc, xc, g.modt], writes=[xc])
            P.D(xt_view(g, tok0, T), xc[:, :, 0:T], reads=[xc], writes=[g.xreg[ci]], eng="act")


def stage_final(P, nc, g):
    with Ctx(nc, P) as cx:
        stage_consts(P, cx, g)
        xc = RR(cx.sb("xc", [128, KC, CH], F32, n=2))
        tmp = cx.sb("tmp", [128, KC, CH], F32)
        sq = cx.sb("sq", [128, KC, CH], BF16)
        rstd = cx.sb("rstd", [128, CH], F32)
        ot = RR(cx.sb("ot", [128, 4, D], F32, n=2))
        pss = RR(cx.ps("pss", 2))
        pst = RR(cx.ps("pst", 4))
        for (tok0, T, r) in CHUNKS:
            if r == 1:
                continue
            ci = tok0 // CH
            x = xc.nxt()
            P.D(x[:, :, 0:T], xt_view(g, tok0, T), reads=[g.xreg[ci]], writes=[x])
            P.I("act", "activation", sq[:, :, 0:T], x[:, :, 0:T], AF.Square, reads=[x], writes=[sq])
            ps = pss.nxt()
            for kc in range(KC):
                P.I("pe", "matmul", ps[:, 0:T], g.ones_b[:], sq[:, kc, 0:T], start=(kc == 0), stop=(kc == KC - 1),
                    reads=[g.ones_b, sq], writes=[ps])
            P.I("act", "activation", rstd[:, 0:T], ps[:, 0:T], AF.Sqrt, bias=g.eps[:, 0:1], scale=1.0 / D, reads=[ps, g.eps], writes=[rstd])
            P.I("dve", "reciprocal", rstd[:, 0:T], rstd[:, 0:T], reads=[rstd], writes=[rstd])
            for kc in range(KC):
                P.I("dve", "scalar_tensor_tensor", tmp[:, kc, 0:T], x[:, kc, 0:T], g.fgT[:, kc:kc + 1], rstd[:, 0:T], ALU.mult, ALU.mult,
                    reads=[x, rstd, g.fgT], writes=[tmp])
            o = ot.nxt()
            for tt in range(T // 128):
                for half in range(2):
                    pt = pst.nxt()
                    for k4 in range(4):
                        kc = half * 4 + k4
                        P.I("pe", "transpose", pt[:, k4 * 128:(k4 + 1) * 128], tmp[:, kc, tt * 128:(tt + 1) * 128], g.ident[:],
                            reads=[tmp, g.ident], writes=[pt])
                    P.I("act" if half else "dve", "copy" if half else "tensor_copy", o[:, tt, half * 512:(half + 1) * 512], pt[:],
                        reads=[pt], writes=[o])
            P.D(g.out[tok0:tok0 + T, :].rearrange("(t p) d -> p t d", p=128), o[:, 0:T // 128, :], reads=[o], writes=[g.outreg])


def stage_attn(P, nc, g, l, j, last):
    SC = 128 ** -0.5
    with Ctx(nc, P) as cx:
        stage_consts(P, cx, g)
        wout = load_w_bf16(P, cx, g.dr["mix_w_out"][l * D:(l + 1) * D, :], D, "wout")
        KT = cx.sb("KT", [128, 2, NT], BF16)
        V = cx.sb("V", [128, NT // 128, 256], BF16)
        with Ctx(nc, P) as c1:
            permT = c1.sb("permT", [128, 128], F32)
            P.D(permT[:], g.dr["permT"], writes=[permT])
            win = load_w_bf16(P, c1, g.dr["attn_w_in"][j * D:(j + 1) * D, :], 1536, "win")
            gains = c1.sb("gains", [128, 2], F32)
            P.D(gains[:, 0:1], g.dr["attn_q_gain"][j:j + 1, :].rearrange("o d -> d o"), writes=[gains], allow_slow_non_contiguous=True)
            P.D(gains[:, 1:2], g.dr["attn_k_gain"][j:j + 1, :].rearrange("o d -> d o"), writes=[gains], allow_slow_non_contiguous=True)
            xc = c1.sb("xc", [128, KC, CH], F32)
            tmp = c1.sb("tmp", [128, KC, CH], F32)
            sq = c1.sb("sq", [128, KC, CH], BF16)
            hb = c1.sb("hb", [128, KC, CH], BF16)
            rstd = c1.sb("rstd", [128, CH], F32)
            cs = RR(c1.sb("cs", [128, 2, CH], F32, n=2))
            sqh = RR(c1.sb("sqh", [128, CH], BF16, n=2))
            rsh = RR(c1.sb("rsh", [128, CH], F32, n=2))
            kn = RR(c1.sb("kn", [128, CH], F32, n=2))
            t1 = RR(c1.sb("t1", [128, CH], F32, n=2))
            t2 = RR(c1.sb("t2", [128, CH], F32, n=2))
            qo = RR([c1.sb("qo", [128, KC, CH], BF16)])
            psa = RR(c1.ps("psa", 3))
            psb = RR(c1.ps("psb", 3))
            psn = RR(c1.ps("psn", 2))

            def head(src_col, T, gcol, rope, dst_ap, dst_tl, cst):
                ps = psa.nxt()
                for kc in range(KC):
                    P.I("pe", "matmul", ps[:, 0:T], win[:, kc, src_col:src_col + 128], hb[:, kc, 0:T],
                        start=(kc == 0), stop=(kc == KC - 1), reads=[win, hb], writes=[ps])
                s2 = sqh.nxt()
                P.I("act", "activation", s2[:, 0:T], ps[:, 0:T], AF.Square, reads=[ps], writes=[s2])
                p2 = psb.nxt()
                P.I("pe", "matmul", p2[:, 0:T], g.ones_b[:], s2[:, 0:T], start=True, stop=True, reads=[g.ones_b, s2], writes=[p2])
                rs = rsh.nxt()
                P.I("act", "activation", rs[:, 0:T], p2[:, 0:T], AF.Sqrt, bias=g.eps[:, 0:1], scale=1.0 / 128, reads=[p2, g.eps], writes=[rs])
                P.I("dve", "reciprocal", rs[:, 0:T], rs[:, 0:T], reads=[rs], writes=[rs])
                k = kn.nxt()
                P.I("dve", "scalar_tensor_tensor", k[:, 0:T], ps[:, 0:T], gains[:, gcol:gcol + 1], rs[:, 0:T], ALU.mult, ALU.mult,
                    reads=[ps, gains, rs], writes=[k])
                if not rope:
                    P.I("act", "copy", dst_ap, k[:, 0:T], reads=[k], writes=[dst_tl])
                    return
                p3 = psb.nxt()
                P.I("pe", "matmul", p3[:, 0:T], permT[:], k[:, 0:T], start=True, stop=True, reads=[permT, k], writes=[p3])
                a, b = t1.nxt(), t2.nxt()
                P.I("pool", "tensor_tensor", a[:, 0:T], k[:, 0:T], cst[:, 0, 0:T], ALU.mult, reads=[k, cst], writes=[a])
                P.I("dve", "tensor_tensor", b[:, 0:T], p3[:, 0:T], cst[:, 1, 0:T], ALU.mult, reads=[p3, cst], writes=[b])
                P.I("pool", "tensor_tensor", dst_ap, a[:, 0:T], b[:, 0:T], ALU.add, reads=[a, b], writes=[dst_tl])

            for (tok0, T, r) in CHUNKS:
                ci = tok0 // CH
                P.D(xc[:, :, 0:T], xt_view(g, tok0, T), reads=[g.xreg[ci]], writes=[xc])
                norm_mod(P, g, xc, T, lambda kc: g.modt[:, l, 8 + kc, r:r + 1], lambda kc: g.modt[:, l, 0 + kc, r:r + 1],
                         sq, rstd, tmp, hb, psn.nxt())
                cst = None
                if r == 0:
                    cst = cs.nxt()
                    P.D(cst[:, 0, 0:T], g.dr["cosT"][:, tok0:tok0 + T], writes=[cst])
                    P.D(cst[:, 1, 0:T], g.dr["sinT"][:, tok0:tok0 + T], writes=[cst])
                for hk in range(2):
                    head(1024 + hk * 128, T, 1, r == 0, KT[:, hk, tok0:tok0 + T], KT, cst)
                for tt in range(T // 128):
                    ps = psa.nxt()
                    for kc in range(KC):
                        P.I("pe", "matmul", ps[:, 0:256], hb[:, kc, tt * 128:(tt + 1) * 128], win[:, kc, 1280:1536],
                            start=(kc == 0), stop=(kc == KC - 1), reads=[hb, win], writes=[ps])
                    P.I("act", "copy", V[:, tok0 // 128 + tt, :], ps[:, 0:256], reads=[ps], writes=[V])
                if r == 1 and last:
                    continue
                q = qo.nxt()
                for h in range(8):
                    head(h * 128, T, 0, r == 0, q[:, h, 0:T], q, cst)
                P.D(g.qT[:, tok0:tok0 + T].rearrange("(k p) t -> p k t", p=128), q[:, :, 0:T], reads=[q], writes=[g.qreg[ci]], eng="act")

        with Ctx(nc, P) as c2:
            xc = RR(c2.sb("xc2", [128, KC, CH], F32, n=2))
            qc = RR(c2.sb("qc", [128, KC, CH], BF16, n=2))
            pT = RR(c2.sb("pT", [128, CH], BF16, n=4))
            y = RR(c2.sb("y", [128, KC, CH], BF16, n=2))
            rec = RR(c2.sb("rec", [128, CH], F32, n=2))
            pss = RR(c2.ps("pss", 3))
            pso = RR(c2.ps("pso", 2))
            psd = RR(c2.ps("psd", 2))
            pspj = RR(c2.ps("pspj", 1))
            for (tok0, T, r) in CHUNKS:
                if r == 1 and last:
                    continue
                ci = tok0 // CH
                kts = list(range(NT // 128)) if r == 0 else list(range(S // 128, NT // 128))
                x = xc.nxt()
                q = qc.nxt()
                P.D(q[:, :, 0:T], g.qT[:, tok0:tok0 + T].rearrange("(k p) t -> p k t", p=128), reads=[g.qreg[ci]], writes=[q])
                P.D(x[:, :, 0:T], xt_view(g, tok0, T), reads=[g.xreg[ci]], writes=[x])
                yy = y.nxt()
                for h in range(8):
                    kv = h // 4
                    po, pd = pso.nxt(), psd.nxt()
                    def pv(ii, kt, p):
                        P.I("pe", "matmul", po[:, 0:T], V[:, kt, kv * 128:(kv + 1) * 128], p[:, 0:T], start=(ii == 0), stop=(ii == len(kts) - 1),
                            reads=[V, p], writes=[po])
                        P.I("pe", "matmul", pd[:, 0:T], g.ones_b[:], p[:, 0:T], start=(ii == 0), stop=(ii == len(kts) - 1),
                            reads=[g.ones_b, p], writes=[pd])

                    pend = None
                    for ii, kt in enumerate(kts):
                        ps = pss.nxt()
                        P.I("pe", "matmul", ps[:, 0:T], KT[:, kv, kt * 128:(kt + 1) * 128], q[:, h, 0:T], start=True, stop=True,
                            reads=[KT, q], writes=[ps])
                        if pend is not None:
                            pv(*pend)
                        p = pT.nxt()
                        P.I("act", "activation", p[:, 0:T], ps[:, 0:T], AF.Exp, scale=SC, reads=[ps], writes=[p])
                        pend = (ii, kt, p)
                    pv(*pend)
                    rc = rec.nxt()
                    P.I("dve", "reciprocal", rc[:, 0:T], pd[:, 0:T], reads=[pd], writes=[rc])
                    P.I("dve", "tensor_tensor", yy[:, h, 0:T], po[:, 0:T], rc[:, 0:T], ALU.mult, reads=[po, rc], writes=[yy])
                out_proj_residual(P, g, l, r, wout, yy, x, T, pspj)
                P.D(xt_view(g, tok0, T), x[:, :, 0:T], reads=[x], writes=[g.xreg[ci]], eng="act")


def stage_gmlp(P, nc, g, l, last):
    with Ctx(nc, P) as cx:
        stage_consts(P, cx, g)
        wout = load_w_bf16(P, cx, g.dr["mix_w_out"][l * D:(l + 1) * D, :], D, "wout")
        win = load_w_bf16(P, cx, g.dr["gmlp_w_in"], 2 * D, "win")
        psA = RR(cx.ps("psA", 3))
        psV = RR(cx.ps("psV", 2))
        psM = RR(cx.ps("psM", 2))
        pspj = RR(cx.ps("pspj", 1))
        wsT = cx.sb("wsT", [128, 8, 128], BF16)
        vgT = cx.sb("vgT", [128, 8, 1], F32)
        bsb = cx.sb("bsb", [128, D], F32)
        P.D(bsb[:], g.dr["gmlp_b_s"][0:1, :].partition_broadcast(128), writes=[bsb])
        with Ctx(nc, P) as c0:
            wsf = c0.sb("wsf", [128, 8, 128], F32)
            P.D(wsf[:], g.dr["gmlp_w_s"].rearrange("(g p) q -> p g q", p=128), writes=[wsf])
            for gi in range(8):
                ps = psA.nxt()
                P.I("pe", "transpose", ps[:, 0:128], wsf[:, gi, :], g.ident[:], reads=[wsf, g.ident], writes=[ps])
                P.I("act", "copy", wsT[:, gi, :], ps[:, 0:128], reads=[ps], writes=[wsT])
            rows_to_cols(P, c0, g, g.dr["gmlp_v_gain"], 1, D, vgT, psA.nxt())
        xc = cx.sb("xc", [128, KC, CH], F32)
        tmp = cx.sb("tmp", [128, KC, CH], F32)
        sq = cx.sb("sq", [128, KC, CH], BF16)
        hb = cx.sb("hb", [128, KC, CH], BF16)
        rstd = cx.sb("rstd", [128, CH], F32)
        uT = cx.sb("uT", [128, KC, CH], F32)
        vg = RR(cx.sb("vg", [128, D], F32, n=2))
        vn = RR(cx.sb("vn", [128, D], BF16, n=2))
        junk = cx.sb("junk", [128, D], BF16)
        st = RR(cx.sb("st", [128, 4], F32, n=2))
        mt = RR(cx.sb("mt", [128, 128], F32, n=2))
        y = cx.sb("y", [128, KC, CH], BF16)
        chunks = [c for c in CHUNKS if not (last and c[2] == 1)]
        for (tok0, T, r) in chunks:
            ci = tok0 // CH
            P.D(xc[:, :, 0:T], xt_view(g, tok0, T), reads=[g.xreg[ci]], writes=[xc])
            norm_mod(P, g, xc, T, lambda kc: g.modt[:, l, 8 + kc, r:r + 1], lambda kc: g.modt[:, l, 0 + kc, r:r + 1],
                     sq, rstd, tmp, hb, psA.nxt())
            for mc in range(KC):
                ps = psA.nxt()
                for kc in range(KC):
                    P.I("pe", "matmul", ps[:, 0:T], win[:, kc, mc * 128:(mc + 1) * 128], hb[:, kc, 0:T],
                        start=(kc == 0), stop=(kc == KC - 1), reads=[win, hb], writes=[ps])
                P.I("act", "activation", uT[:, mc, 0:T], ps[:, 0:T], AF.Gelu, reads=[ps], writes=[uT])
            for tt in range(T // 128):
                v = vg.nxt()
                for half in range(2):
                    ps = psV.nxt()
                    for kc in range(KC):
                        P.I("pe", "matmul", ps[:], hb[:, kc, tt * 128:(tt + 1) * 128], win[:, kc, D + half * 512:D + (half + 1) * 512],
                            start=(kc == 0), stop=(kc == KC - 1), reads=[hb, win], writes=[ps])
                    P.I("act", "activation", v[:, half * 512:(half + 1) * 512], ps[:], AF.Gelu, reads=[ps], writes=[v])
                s4 = st.nxt()
                P.I("dve", "memset", s4[:], 0.0, writes=[s4])
                P.I("act", "activation", junk[:], v[:], AF.Square, accum_out=s4[:, 0:1], reads=[v, s4], writes=[junk, s4])
                P.I("act", "activation", s4[:, 1:2], s4[:, 0:1], AF.Sqrt, bias=g.eps[:, 0:1], scale=1.0 / D, reads=[s4, g.eps], writes=[s4])
                P.I("dve", "reciprocal", s4[:, 2:3], s4[:, 1:2], reads=[s4], writes=[s4])
                n = vn.nxt()
                P.I("dve", "tensor_scalar", n[:], v[:], s4[:, 2:3], None, ALU.mult, reads=[v, s4], writes=[n])
                for half in range(2):
                    pm = psM.nxt()
                    for g4 in range(4):
                        gi = half * 4 + g4
                        P.I("pe", "matmul", pm[:, g4 * 128:(g4 + 1) * 128], n[:, gi * 128:(gi + 1) * 128], wsT[:, gi, :], start=True, stop=True,
                            reads=[n, wsT], writes=[pm])
                    for g4 in range(4):
                        gi = half * 4 + g4
                        m = mt.nxt()
                        P.I("dve", "scalar_tensor_tensor", m[:], pm[:, g4 * 128:(g4 + 1) * 128], vgT[:, gi, 0:1], bsb[:, gi * 128:(gi + 1) * 128],
                            ALU.mult, ALU.add, reads=[pm, vgT, bsb], writes=[m])
                        P.I("pool", "tensor_tensor", y[:, gi, tt * 128:(tt + 1) * 128], m[:], uT[:, gi, tt * 128:(tt + 1) * 128], ALU.mult,
                            reads=[m, uT], writes=[y])
            out_proj_residual(P, g, l, r, wout, y, xc, T, pspj)
            P.D(xt_view(g, tok0, T), xc[:, :, 0:T], reads=[xc], writes=[g.xreg[ci]], eng="act")


def build_program(layers=(0, 1, 2, 3), do_final=True, dbg_x=False, skip_moe=False, skip_mixer=False):
    nc = bass.Bass("TRN2", target_bir_lowering=False)
    g = G()
    g.dr = {}
    g.dr["x"] = nc.dram_tensor("x", [S, D], F32, kind="ExternalInput").ap()
    g.dr["ctx"] = nc.dram_tensor("ctx", [C, D], F32, kind="ExternalInput").ap()
    g.dr["cvec"] = nc.dram_tensor("cvec", [2, D], F32, kind="ExternalInput").ap()
    for k, shp in WEIGHT_SHAPES.items():
        g.dr[k] = nc.dram_tensor(k, list(shp), F32, kind="ExternalInput").ap()
    for k, shp in CONST_SHAPES.items():
        g.dr[k] = nc.dram_tensor(k, list(shp), F32, kind="ExternalInput").ap()
    g.out = nc.dram_tensor("out", [S, D], F32, kind="ExternalOutput").ap()
    if dbg_x:
        g.xT = nc.dram_tensor("xT_d", [D, NT], F32, kind="ExternalOutput").ap()
    else:
        g.xT = nc.dram_tensor("xT_d", [D, NT], F32).ap()
    g.qT = nc.dram_tensor("qT_d", [D, NT], BF16).ap()
    g.vT = nc.dram_tensor("vT_d", [D, NT], F32).ap()
    g.x0T = nc.dram_tensor("x0T_d", [D, NT], F32).ap()
    g.ycT = nc.dram_tensor("ycT_d", [D, NT], F32).ap()
    g.fT = nc.dram_tensor("fT_d", [D, 2 * S], F32).ap()
    g.freg = Tl(None, "freg")
    g.vreg = Tl(None, "vreg")
    g.yreg = Tl(None, "yreg")
    g.w1b = [nc.dram_tensor(f"w1b_d{l}", [NE * D, 2 * D], BF16).ap() if (l in layers and not skip_moe) else None for l in range(DEPTH)]
    g.w2b = [nc.dram_tensor(f"w2b_d{l}", [NE * D, D], BF16).ap() if (l in layers and not skip_moe) else None for l in range(DEPTH)]
    g.xreg = [Tl(None, f"xreg{i}") for i in range(len(CHUNKS))]
    g.qreg = [Tl(None, f"qreg{i}") for i in range(len(CHUNKS))]
    g.wreg = [Tl(None, f"wreg{i}") for i in range(DEPTH)]
    g.outreg = Tl(None, "outreg")
    with contextlib.ExitStack() as st:
        P = Prog(nc, st)
        g.modt = Tl(st.enter_context(nc.sbuf_tensor("modt", [128, DEPTH, 48, 2], F32)), "modt")
        g.fgT = Tl(st.enter_context(nc.sbuf_tensor("fgT", [128, KC], F32)), "fgT")
        stage_x_in(P, nc, g)
        stage_mods(P, nc, g, layers)
        if not skip_moe:
            stage_cast_experts(P, nc, g, layers)
        for l in layers:
            kind, j, last = l % 3, l // 3, l == DEPTH - 1
            if not skip_mixer:
                if kind == 0:
                    stage_attn(P, nc, g, l, j, last)
                elif kind == 1:
                    stage_gmlp(P, nc, g, l, last)
                else:
                    stage_hyena(P, nc, g, l, last)
            if not skip_moe:
                stage_moe(P, nc, g, l, last)
        if do_final:
            stage_final(P, nc, g)
        P.final_wait("sync")
        P.emit()
    return nc


def make_in_maps(inputs, cores):
    consts = _consts()
    shared = {}
    for k, shp in WEIGHT_SHAPES.items():
        shared[k] = np.ascontiguousarray(np.asarray(inputs[k], dtype=np.float32).reshape(shp))
    shared.update(consts)
    maps = []
    for b in cores:
        m = dict(shared)
        m["x"] = np.ascontiguousarray(inputs["x"][b])
        m["ctx"] = np.ascontiguousarray(inputs["ctx"][b])
        m["cvec"] = np.ascontiguousarray(np.stack([inputs["c"][b], inputs["c_ctx"]], axis=0).astype(np.float32))
        maps.append(m)
    return maps


def kernel(**inputs):
    nc = build_program()
    maps = make_in_maps(inputs, list(range(8)))
    res = run_bass_kernel_spmd(nc, maps, core_ids=list(range(8)))
    return np.stack([r["out"] for r in res.results], axis=0).astype(np.float32)


HCH = 510


def _hyena_consts(L):
    t = np.linspace(0.0, 1.0, L, dtype=np.float32)
    w = (2.0 * math.pi * np.arange(L, dtype=np.float32) / L).astype(np.float32)
    f = np.linspace(1e-4, 15, 16, dtype=np.float32)[None, :]
    z = np.concatenate([t[:, None], np.cos(f * w[:, None]), -np.sin(f * w[:, None])], axis=-1).astype(np.float32)
    pos = np.abs((L - 1) - np.arange(2 * L))
    pos = np.minimum(pos, L - 1)
    zH = np.ascontiguousarray(z[pos].T)
    tH = np.ascontiguousarray(t[pos][None, :])
    return zH, tH


def _hyena_consts_nat(L):
    t = np.linspace(0.0, 1.0, L, dtype=np.float32)
    w = (2.0 * math.pi * np.arange(L, dtype=np.float32) / L).astype(np.float32)
    f = np.linspace(1e-4, 15, 16, dtype=np.float32)[None, :]
    z = np.concatenate([t[:, None], np.cos(f * w[:, None]), -np.sin(f * w[:, None])], axis=-1).astype(np.float32)
    n = np.arange(2 * L)
    pos = np.where(n < L, n, 2 * L - n)
    pos = np.minimum(pos, L - 1)
    zN = np.ascontiguousarray(z[pos].T)
    tN = t[pos].copy()
    tN[L] = 1.0e4
    return zN, np.ascontiguousarray(tN[None, :])


def stage_hyena(P, nc, g, l, last):
    TWO_PI = 2.0 * math.pi
    with Ctx(nc, P) as cx:
        stage_consts(P, cx, g)
        with Ctx(nc, P) as c1:
            win = load_w_bf16(P, c1, g.dr["hyena_w_in"], 3 * D, "win")
            psA = RR(c1.ps("psA", 4))
            psn = RR(c1.ps("psn", 2))
            cwT = c1.sb("cwT", [128, 24, 3], F32)
            cbT = c1.sb("cbT", [128, 24, 1], F32)
            with Ctx(nc, P) as c0:
                rows_to_cols(P, c0, g, g.dr["hyena_conv_w"], 3, 3 * D, cwT, psA.nxt())
                rows_to_cols(P, c0, g, g.dr["hyena_conv_b"], 1, 3 * D, cbT, psA.nxt())
            xh = c1.sb("xh", [128, KC, 512], F32)
            tmp = c1.sb("tmp", [128, KC, 512], F32)
            sq = c1.sb("sq", [128, KC, 512], BF16)
            hb = c1.sb("hb", [128, KC, 512], BF16)
            rstd = c1.sb("rstd", [128, 512], F32)
            pc = RR(c1.sb("pc", [128, 512], F32, n=2))
            ca = RR(c1.sb("ca", [128, 512], F32, n=2))
            x1c = c1.sb("x1c", [128, KC, 512], F32)
            x0c = c1.sb("x0c", [128, KC, 512], F32)
            vvc = c1.sb("vvc", [128, KC, 512], F32)
            seqs = [(0, S, 0)] + ([] if last else [(S, C, 1)])
            for (s0, L, r) in seqs:
                tok0 = s0
                while tok0 < s0 + L:
                    T = min(HCH, s0 + L - tok0)
                    lo_ok = tok0 - 1 >= s0
                    hi_ok = tok0 + T < s0 + L
                    W = T + 2
                    c_lo = tok0 - 1 if lo_ok else tok0
                    c_hi = tok0 + T + 1 if hi_ok else tok0 + T
                    regs = [g.xreg[i] for i in range(len(CHUNKS)) if CHUNKS[i][0] < c_hi and CHUNKS[i][0] + CHUNKS[i][1] > c_lo]
                    if not (lo_ok and hi_ok):
                        P.I("pool", "memset", xh[:, :, 0:W], 1.0, writes=[xh])
                    P.D(xh[:, :, (c_lo - (tok0 - 1)):(c_hi - (tok0 - 1))], g.xT[:, c_lo:c_hi].rearrange("(k p) t -> p k t", p=128),
                        reads=regs, writes=[xh])
                    norm_mod(P, g, xh, W, lambda kc: g.modt[:, l, 8 + kc, r:r + 1], lambda kc: g.modt[:, l, 0 + kc, r:r + 1],
                             sq, rstd, tmp, hb, psn.nxt())
                    for m in list(range(8, 16)) + list(range(0, 8)) + list(range(16, 24)):
                        ps = psA.nxt()
                        for kc in range(KC):
                            P.I("pe", "matmul", ps[:, 0:W], win[:, kc, m * 128:(m + 1) * 128], hb[:, kc, 0:W],
                                start=(kc == 0), stop=(kc == KC - 1), reads=[win, hb], writes=[ps])
                        p = pc.nxt()
                        P.I("act", "copy", p[:, 0:W], ps[:, 0:W], reads=[ps], writes=[p])
                        if not lo_ok:
                            P.I("pool", "memset", p[:, 0:1], 0.0, writes=[p])
                        if not hi_ok:
                            P.I("pool", "memset", p[:, W - 1:W], 0.0, writes=[p])
                        a = ca.nxt()
                        P.I("dve", "tensor_scalar", a[:, 0:T], p[:, 0:T], cwT[:, m, 0:1], cbT[:, m, 0:1], ALU.mult, ALU.add,
                            reads=[p, cwT, cbT], writes=[a])
                        P.I("dve", "scalar_tensor_tensor", a[:, 0:T], p[:, 1:T + 1], cwT[:, m, 1:2], a[:, 0:T], ALU.mult, ALU.add,
                            reads=[p, cwT, a], writes=[a])
                        if m < 8:
                            dst, dtl = x0c[:, m, 0:T], x0c
                        elif m < 16:
                            dst, dtl = x1c[:, m - 8, 0:T], x1c
                        else:
                            dst, dtl = a[:, 0:T], a
                        P.I("dve", "scalar_tensor_tensor", dst, p[:, 2:T + 2], cwT[:, m, 2:3], a[:, 0:T], ALU.mult, ALU.add,
                            reads=[p, cwT, a], writes=[dtl])
                        if m >= 16:
                            P.I("pool", "tensor_tensor", vvc[:, m - 16, 0:T], a[:, 0:T], x1c[:, m - 16, 0:T], ALU.mult,
                                reads=[a, x1c], writes=[vvc])
                    P.D(g.vT[:, tok0:tok0 + T].rearrange("(k p) t -> p k t", p=128), vvc[:, :, 0:T], reads=[vvc], writes=[g.vreg], eng="act")
                    P.D(g.x0T[:, tok0:tok0 + T].rearrange("(k p) t -> p k t", p=128), x0c[:, :, 0:T], reads=[x0c], writes=[g.vreg], eng="act")
                    tok0 += T

        for (s0, L, r) in seqs:
            if r == 0:
                hyena_conv_fft(P, nc, g)
            else:
                hyena_conv_direct(P, nc, g, s0, L)

        with Ctx(nc, P) as c4:
            wout = load_w_bf16(P, c4, g.dr["mix_w_out"][l * D:(l + 1) * D, :], D, "wout")
            dT = c4.sb("dT", [128, KC, 1], F32)
            pspj = RR(c4.ps("pspj", 2))
            with Ctx(nc, P) as c0:
                rows_to_cols(P, c0, g, g.dr["hyena_d"], 1, D, dT, pspj.nxt())
            xc = RR(c4.sb("xc", [128, KC, CH], F32, n=2))
            yc = RR(c4.sb("ycc", [128, KC, CH], F32, n=2))
            vc = RR(c4.sb("vcc", [128, KC, CH], F32, n=2))
            x0 = RR(c4.sb("x0cc", [128, KC, CH], F32, n=2))
            y = RR(c4.sb("y", [128, KC, CH], BF16, n=2))
            for (tok0, T, r) in CHUNKS:
                if r == 1 and last:
                    continue
                ci = tok0 // CH
                x, yy, a_, b_, c_ = xc.nxt(), y.nxt(), yc.nxt(), vc.nxt(), x0.nxt()
                fm = lambda ap: ap[:, tok0:tok0 + T].rearrange("(k p) t -> p k t", p=128)
                P.D(x[:, :, 0:T], xt_view(g, tok0, T), reads=[g.xreg[ci]], writes=[x])
                P.D(a_[:, :, 0:T], fm(g.ycT), reads=[g.yreg], writes=[a_])
                P.D(b_[:, :, 0:T], fm(g.vT), reads=[g.vreg], writes=[b_])
                P.D(c_[:, :, 0:T], fm(g.x0T), reads=[g.vreg], writes=[c_])
                for kc in range(KC):
                    P.I("dve", "scalar_tensor_tensor", a_[:, kc, 0:T], b_[:, kc, 0:T], dT[:, kc, 0:1], a_[:, kc, 0:T], ALU.mult, ALU.add,
                        reads=[a_, b_, dT], writes=[a_])
                P.I("pool", "tensor_tensor", yy[:, :, 0:T], a_[:, :, 0:T], c_[:, :, 0:T], ALU.mult, reads=[a_, c_], writes=[yy])
                out_proj_residual(P, g, l, r, wout, yy, x, T, pspj)
                P.D(xt_view(g, tok0, T), x[:, :, 0:T], reads=[x], writes=[g.xreg[ci]], eng="act")


def hyena_filter_mlp(P, nc, g, c3, a3, zH_d, L2, CW, psF):
    TWO_PI = 2.0 * math.pi
    fw = [c3.sb("fw1", [33, 64], F32), c3.sb("fw2", [64, 64], F32), c3.sb("fw3", [64, 64], F32)]
    P.D(fw[0][:], g.dr["hyena_f_w1"], writes=[fw[0]])
    P.D(fw[1][:], g.dr["hyena_f_w2"], writes=[fw[1]])
    P.D(fw[2][:], g.dr["hyena_f_w3"], writes=[fw[2]])
    fb = c3.sb("fb", [64, 4], F32)
    for i, nm in enumerate(["hyena_f_b1", "hyena_f_b2", "hyena_f_b3", "hyena_freq"]):
        P.D(fb[:, i:i + 1], g.dr[nm].rearrange("o d -> d o"), writes=[fb], allow_slow_non_contiguous=True)
    P.I("dve", "tensor_scalar", fb[:, 3:4], fb[:, 3:4], 1.0 / TWO_PI, None, ALU.mult, reads=[fb], writes=[fb])
    zt = RR(c3.sb("zt", [33, CW], F32, n=2))
    u = RR(c3.sb("u", [64, CW], F32, n=2))
    ki = RR(c3.sb("ki", [64, CW], I32, n=2))
    kf = RR(c3.sb("kf", [64, CW], F32, n=2))
    cmpb = RR(c3.sb("cmpb", [64, CW], F32, n=2))
    av = RR(c3.sb("av", [64, CW], F32, n=2))
    for n0 in range(0, L2, CW):
        z = zt.nxt()
        P.D(z[:], zH_d[:, n0:n0 + CW], writes=[z])
        cur, K = z, 33
        for li in range(3):
            ps = psF.nxt()
            P.I("pe", "matmul", ps[0:64, 0:CW], fw[li][0:K, :], cur[0:K, :], start=True, stop=True, reads=[fw[li], cur], writes=[ps])
            uu, kk, ff, cc = u.nxt(), ki.nxt(), kf.nxt(), cmpb.nxt()
            P.I("dve", "tensor_scalar", uu[:], ps[0:64, 0:CW], fb[:, li:li + 1], fb[:, 3:4], ALU.add, ALU.mult, reads=[ps, fb], writes=[uu])
            P.I("dve", "tensor_copy", kk[:], uu[:], reads=[uu], writes=[kk])
            P.I("dve", "tensor_copy", ff[:], kk[:], reads=[kk], writes=[ff])
            P.I("dve", "tensor_tensor", uu[:], uu[:], ff[:], ALU.subtract, reads=[uu, ff], writes=[uu])
            P.I("dve", "tensor_scalar", cc[:], uu[:], 0.5, None, ALU.is_gt, reads=[uu], writes=[cc])
            P.I("dve", "tensor_tensor", uu[:], uu[:], cc[:], ALU.subtract, reads=[uu, cc], writes=[uu])
            P.I("dve", "tensor_scalar", cc[:], uu[:], -0.5, None, ALU.is_lt, reads=[uu], writes=[cc])
            P.I("dve", "tensor_tensor", uu[:], uu[:], cc[:], ALU.add, reads=[uu, cc], writes=[uu])
            if li < 2:
                nx = av.nxt()
                P.I("act", "activation", nx[:], uu[:], AF.Sin, scale=TWO_PI * (1.0 - 1e-6), reads=[uu], writes=[nx])
                cur, K = nx, 64
            else:
                P.I("act", "activation", a3[:, n0:n0 + CW], uu[:], AF.Sin, scale=TWO_PI * (1.0 - 1e-6), reads=[uu], writes=[a3])


def hyena_filter_chunk(P, g, psF, w4b, a3, b4T, ndl, tb, wn, tH_d, cc, n0, CW, L, dst_ap, dst_tl):
    half = 0 if n0 < L else 1
    ps = psF.nxt()
    P.I("pe", "matmul", ps[:, 0:CW], w4b[:, half * D + cc * 128: half * D + (cc + 1) * 128], a3[:, n0:n0 + CW],
        start=True, stop=True, reads=[w4b, a3], writes=[ps])
    t_ = tb.nxt()
    P.D(t_[:, 0:CW], tH_d[0:1, n0:n0 + CW].partition_broadcast(128), writes=[t_])
    w_ = wn.nxt()
    P.I("act", "activation", w_[:, 0:CW], t_[:, 0:CW], AF.Exp, scale=ndl[:, cc:cc + 1], reads=[t_, ndl], writes=[w_])
    P.I("dve", "scalar_tensor_tensor", dst_ap, ps[:, 0:CW], b4T[:, half * 8 + cc, 0:1], w_[:, 0:CW], ALU.add, ALU.mult,
        reads=[ps, b4T, w_], writes=[dst_tl])


def hyena_conv_direct(P, nc, g, s0, L):
    L2, CW = 2 * L, min(512, L)
    with Ctx(nc, P) as c2:
        psF = RR(c2.ps("psF", 4))
        a3 = c2.sb("a3", [64, L2], BF16)
        w4b = c2.sb("w4b", [64, 2 * D], BF16)
        P.D(w4b[:], g.dr["hyena_f_w4"], writes=[w4b], eng="pool")
        b4T = c2.sb("b4T", [128, 16, 1], F32)
        ndl = c2.sb("ndl", [128, KC], F32)
        P.D(ndl[:], g.dr["negdelta"], writes=[ndl])
        with Ctx(nc, P) as c0:
            rows_to_cols(P, c0, g, g.dr["hyena_f_b4"], 1, 2 * D, b4T, psF.nxt())
        with Ctx(nc, P) as c3:
            hyena_filter_mlp(P, nc, g, c3, a3, g.dr["zH_ctx"], L2, CW, psF)
        H = c2.sb("H", [128, L2], BF16)
        vt = c2.sb("vt", [128, L], BF16)
        junk = c2.sb("junk", [128, L], BF16)
        ycol = RR(c2.sb("ycol", [128, L], F32, n=2))
        tb = RR(c2.sb("tb", [128, CW], F32, n=2))
        wn = RR(c2.sb("wn", [128, CW], F32, n=2))
        for cc in range(KC):
            P.D(vt[:], g.vT[cc * 128:(cc + 1) * 128, s0:s0 + L], reads=[g.vreg], writes=[vt], eng="pool")
            for n0 in range(0, L2, CW):
                hyena_filter_chunk(P, g, psF, w4b, a3, b4T, ndl, tb, wn, g.dr["tH_ctx"], cc, n0, CW, L, H[:, n0:n0 + CW], H)
            yc_ = ycol.nxt()
            for t in range(L):
                P.I("dve", "scalar_tensor_tensor", junk[:], H[:, L - 1 - t:2 * L - 1 - t], 1.0, vt[:], ALU.mult, ALU.mult,
                    accum_out=yc_[:, t:t + 1], reads=[H, vt], writes=[junk, yc_])
            P.D(g.ycT[cc * 128:(cc + 1) * 128, s0:s0 + L], yc_[:], reads=[yc_], writes=[g.yreg], eng="act")


def hyena_conv_fft(P, nc, g):
    L, L2, CW = S, 2 * S, 512
    NFFT = float(L2)
    with Ctx(nc, P) as c2:
        psF = RR(c2.ps("psF", 4))
        a3 = c2.sb("a3", [64, L2], BF16)
        w4b = c2.sb("w4b", [64, 2 * D], BF16)
        P.D(w4b[:], g.dr["hyena_f_w4"], writes=[w4b], eng="pool")
        b4T = c2.sb("b4T", [128, 16, 1], F32)
        ndl = c2.sb("ndl", [128, KC], F32)
        P.D(ndl[:], g.dr["negdelta"], writes=[ndl])
        with Ctx(nc, P) as c0:
            rows_to_cols(P, c0, g, g.dr["hyena_f_b4"], 1, 2 * D, b4T, psF.nxt())
        with Ctx(nc, P) as c3:
            hyena_filter_mlp(P, nc, g, c3, a3, g.dr["zN_lat"], L2, CW, psF)
        Hc = RR(c2.sb("Hc", [128, 2048], F32, n=2))
        tb = RR(c2.sb("tb", [128, CW], F32, n=2))
        wn = RR(c2.sb("wn", [128, CW], F32, n=2))
        for cc in range(KC):
            for b0 in range(0, L2, 2048):
                h = Hc.nxt()
                for n0 in range(b0, b0 + 2048, CW):
                    hyena_filter_chunk(P, g, psF, w4b, a3, b4T, ndl, tb, wn, g.dr["tN_lat"], cc, n0, CW, L, h[:, n0 - b0:n0 - b0 + CW], h)
                P.D(g.fT[cc * 128:(cc + 1) * 128, b0:b0 + 2048], h[:], reads=[h], writes=[g.freg], eng="act")

    with Ctx(nc, P) as c5:
        cS = c5.sb("dftCf", [128, 2, 128], F32)
        P.D(cS[:, 0, :], g.dr["dftC"], writes=[cS])
        P.D(cS[:, 1, :], g.dr["dftS"], writes=[cS])
        F1 = c5.sb("F1", [128, 256], BF16)
        G1 = c5.sb("G1", [128, 256], BF16)
        G2 = c5.sb("G2", [128, 256], BF16)
        P.I("dve", "tensor_copy", F1[:, 0:128], cS[:, 0, :], reads=[cS], writes=[F1])
        P.I("dve", "tensor_scalar", F1[:, 128:256], cS[:, 1, :], -1.0, None, ALU.mult, reads=[cS], writes=[F1])
        P.I("dve", "tensor_copy", G1[:, 0:128], cS[:, 0, :], reads=[cS], writes=[G1])
        P.I("dve", "tensor_copy", G1[:, 128:256], cS[:, 1, :], reads=[cS], writes=[G1])
        P.I("dve", "tensor_scalar", G2[:, 0:128], cS[:, 1, :], -1.0, None, ALU.mult, reads=[cS], writes=[G2])
        P.I("dve", "tensor_copy", G2[:, 128:256], cS[:, 0, :], reads=[cS], writes=[G2])
        twC = c5.sb("twC", [128, 512], F32)
        twS = c5.sb("twS", [128, 512], F32)
        P.D(twC[:], g.dr["twC"], writes=[twC])
        P.D(twS[:], g.dr["twS"], writes=[twS])
        Cb, Sb, nSb, nSb2 = G1[:, 0:128], G1[:, 128:256], G2[:, 0:128], F1[:, 128:256]
        ps1 = RR(c5.ps("ps1", 3))
        ps3 = RR(c5.ps("ps3", 2))
        ps4 = RR(c5.ps("ps4", 2))
        ps6 = RR(c5.ps("ps6", 1))
        xf = c5.sb("xf", [64, 32, 128], F32)
        ff = c5.sb("ff", [128, 32, 128], F32)
        xb = RR(c5.sb("xb", [64, 32, 128], BF16, n=2))
        fbb = RR(c5.sb("fbb", [128, 32, 128], BF16, n=2))
        yo = RR(c5.sb("yo", [64, 32, 128], F32, n=2))
        m1 = RR(c5.sb("m1", [128, 512], F32, n=2))
        m2 = RR(c5.sb("m2", [128, 512], F32, n=2))
        pr = RR(c5.sb("pr", [128, 4, 128], BF16, n=3))
        pi = RR(c5.sb("pi", [128, 4, 128], BF16, n=3))
        kfr = RR(c5.sb("kfr", [128, 512], F32, n=2))
        kfi = RR(c5.sb("kfi", [128, 512], F32, n=2))
        ta = RR(c5.sb("ta", [128, 512], F32, n=2))
        tbb = RR(c5.sb("tbb", [128, 512], F32, n=2))
        tc = RR(c5.sb("tc", [128, 512], F32, n=2))
        td = RR(c5.sb("td", [128, 512], F32, n=2))

        def v4(ap):
            return ap.rearrange("p (c r f) -> p c r f", c=2, r=2)

        def twiddle(banks, outR, outI, inverse):
            for bi, ps in enumerate(banks):
                a, b = m1.nxt(), m2.nxt()
                P.I("dve", "tensor_tensor", a[:], ps[:, 0:512], twC[:], ALU.mult, reads=[ps, twC], writes=[a])
                P.I("dve", "tensor_tensor", b[:], ps[:, 0:512], twS[:], ALU.mult, reads=[ps, twS], writes=[b])
                av, bv = v4(a[:]), v4(b[:])
                oR, oI = outR[:, bi * 2:bi * 2 + 2, :], outI[:, bi * 2:bi * 2 + 2, :]
                if not inverse:
                    P.I("pool", "tensor_tensor", oR, av[:, :, 0, :], bv[:, :, 1, :], ALU.add, reads=[a, b], writes=[outR])
                    P.I("pool", "tensor_tensor", oI, av[:, :, 1, :], bv[:, :, 0, :], ALU.subtract, reads=[a, b], writes=[outI])
                else:
                    P.I("pool", "tensor_tensor", oR, av[:, :, 0, :], bv[:, :, 1, :], ALU.subtract, reads=[a, b], writes=[outR])
                    P.I("pool", "tensor_tensor", oI, bv[:, :, 0, :], av[:, :, 1, :], ALU.add, reads=[a, b], writes=[outI])

        def fwd(src, K, c0):
            banks = [ps1.nxt(), ps1.nxt()]
            for ch in range(4):
                P.I("pe", "matmul", banks[ch // 2][:, (ch % 2) * 256:(ch % 2) * 256 + 256], src[0:K, c0 + ch, :], F1[0:K, :],
                    start=True, stop=True, reads=[src, F1], writes=[banks[ch // 2]])
            R, I_ = pr.nxt(), pi.nxt()
            twiddle(banks, R, I_, False)
            Xr, Xi = ps3.nxt(), ps3.nxt()
            Rf, If = R[:].rearrange("p c f -> p (c f)"), I_[:].rearrange("p c f -> p (c f)")
            P.I("pe", "matmul", Xr[:, 0:512], Cb, Rf, start=True, stop=False, reads=[G1, R], writes=[Xr])
            P.I("pe", "matmul", Xr[:, 0:512], Sb, If, start=False, stop=True, reads=[G1, I_], writes=[Xr])
            P.I("pe", "matmul", Xi[:, 0:512], Cb, If, start=True, stop=False, reads=[G1, I_], writes=[Xi])
            P.I("pe", "matmul", Xi[:, 0:512], nSb, Rf, start=False, stop=True, reads=[G2, R], writes=[Xi])
            return Xr, Xi

        for sg in range(D // 32):
            c0 = sg * 32
            P.D(xf[:], g.vT[c0:c0 + 32, 0:S].rearrange("c (a b) -> a c b", b=128), reads=[g.vreg], writes=[xf])
            P.D(ff[:], g.fT[c0:c0 + 32, :].rearrange("c (a b) -> a c b", b=128), reads=[g.freg], writes=[ff])
            x_, f_, y_ = xb.nxt(), fbb.nxt(), yo.nxt()
            P.I("act", "copy", x_[:], xf[:], reads=[xf], writes=[x_])
            P.I("pool", "tensor_copy", f_[:], ff[:], reads=[ff], writes=[f_])
            for gq in range(8):
                Xr, Xi = fwd(f_, 128, gq * 4)
                kr, ki_ = kfr.nxt(), kfi.nxt()
                P.I("act", "activation", kr[:], Xr[:, 0:512], AF.Identity, scale=1.0 / NFFT, reads=[Xr], writes=[kr])
                P.I("act", "activation", ki_[:], Xi[:, 0:512], AF.Identity, scale=1.0 / NFFT, reads=[Xi], writes=[ki_])
                Xr, Xi = fwd(x_, 64, gq * 4)
                a, b, c_, d_ = ta.nxt(), tbb.nxt(), tc.nxt(), td.nxt()
                P.I("dve", "tensor_tensor", a[:], Xr[:, 0:512], kr[:], ALU.mult, reads=[Xr, kr], writes=[a])
                P.I("dve", "tensor_tensor", b[:], Xi[:, 0:512], ki_[:], ALU.mult, reads=[Xi, ki_], writes=[b])
                P.I("dve", "tensor_tensor", c_[:], Xr[:, 0:512], ki_[:], ALU.mult, reads=[Xr, ki_], writes=[c_])
                P.I("dve", "tensor_tensor", d_[:], Xi[:, 0:512], kr[:], ALU.mult, reads=[Xi, kr], writes=[d_])
                Yr, Yi = pr.nxt(), pi.nxt()
                P.I("pool", "tensor_tensor", Yr[:].rearrange("p c f -> p (c f)"), a[:], b[:], ALU.subtract, reads=[a, b], writes=[Yr])
                P.I("pool", "tensor_tensor", Yi[:].rearrange("p c f -> p (c f)"), c_[:], d_[:], ALU.add, reads=[c_, d_], writes=[Yi])
                banks = [ps4.nxt(), ps4.nxt()]
                for ch in range(4):
                    o = banks[ch // 2][:, (ch % 2) * 256:(ch % 2) * 256 + 256]
                    P.I("pe", "matmul", o, Yr[:, ch, :], G1[:], start=True, stop=False, reads=[Yr, G1], writes=[banks[ch // 2]])
                    P.I("pe", "matmul", o, Yi[:, ch, :], G2[:], start=False, stop=True, reads=[Yi, G2], writes=[banks[ch // 2]])
                Qr, Qi = pr.nxt(), pi.nxt()
                twiddle(banks, Qr, Qi, True)
                po = ps6.nxt()
                P.I("pe", "matmul", po[0:64, 0:512], G1[:, 0:64], Qr[:].rearrange("p c f -> p (c f)"), start=True, stop=False,
                    reads=[G1, Qr], writes=[po])
                P.I("pe", "matmul", po[0:64, 0:512], G2[:, 0:64], Qi[:].rearrange("p c f -> p (c f)"), start=False, stop=True,
                    reads=[G2, Qi], writes=[po])
                P.I("act", "copy", y_[:, gq * 4:gq * 4 + 4, :], po[0:64, 0:512].rearrange("p (c f) -> p c f", c=4), reads=[po], writes=[y_])
            P.D(g.ycT[c0:c0 + 32, 0:S].rearrange("c (a b) -> a c b", b=128), y_[:], reads=[y_], writes=[g.yreg], eng="act")
```
